# Optimizing a Trainium2 kernel written in Bass

```python
import jax, jax.numpy as jnp
from jax import lax
import numpy as np

D_MODEL = 1024
BATCH = 8
SEQ = 4096
DEPTH = 4

GRID_W = 64
CTX_LEN = 256
NORM_EPS = 1e-6
N_MOD = 6
D_RNN = 1024
LRU_HEADS = 16
LRU_BW = D_RNN // LRU_HEADS
LRU_C = 8.0
CONV_W = 4
CONV_LEFT = 2
RET_HEADS = 8
RET_DK = 64
RET_DV = 128
RET_CHUNK = 128
ATT_HEADS = 16
ATT_KV_HEADS = 4
ATT_HD = 64
ATT_WIN = 128
ATT_BLOCK = 128
ROPE_BASE = 10000.0
N_GROUPS = 4
EXPERTS_PER_GROUP = 8
N_EXPERTS = N_GROUPS * EXPERTS_PER_GROUP
TOP_K = 2
D_EXPERT = 512
MOE_BLOCK = 128
N_BRANCHES = 3
IN_SPLITS = (D_RNN, D_RNN, RET_HEADS * RET_DK, RET_HEADS * RET_DK, RET_HEADS * RET_DV,
             RET_HEADS * RET_DV, ATT_HEADS * ATT_HD, ATT_KV_HEADS * ATT_HD, ATT_KV_HEADS * ATT_HD,
             N_BRANCHES * D_MODEL)
D_IN = sum(IN_SPLITS)
CTX_STATE_PARTS = (0, 3, 4, 7, 8)

kernel_name = 'hybrid_lru_retention_swa_hmoe_dit'


def split_cols(p):
    idx = [int(v) for v in np.cumsum(IN_SPLITS)[:-1]]
    return jnp.split(p, idx, axis=-1)


def rmsnorm(x, g):
    xf = x.astype(jnp.float32)
    y = xf * lax.rsqrt(jnp.mean(xf * xf, axis=-1, keepdims=True) + NORM_EPS)
    return (y * g.astype(jnp.float32)).astype(x.dtype)


def modulate(h, shift, scale):
    return h * (1 + scale) + shift


def rope_angles(pos, dim):
    inv = ROPE_BASE ** (-(jnp.arange(0, dim, 2, dtype=jnp.float32) / dim))
    return pos[:, None] * inv[None, :]


def apply_rope(x, ang):
    half = x.shape[-1] // 2
    shape = (1, ang.shape[0]) + (1,) * (x.ndim - 3) + (half,)
    cos = jnp.cos(ang).reshape(shape)
    sin = jnp.sin(ang).reshape(shape)
    xf = x.astype(jnp.float32)
    x1, x2 = xf[..., :half], xf[..., half:]
    return jnp.concatenate([x1 * cos - x2 * sin, x2 * cos + x1 * sin], axis=-1).astype(x.dtype)


def axial_rope(x, ang_row, ang_col):
    h = ATT_HD // 2
    return jnp.concatenate([apply_rope(x[..., :h], ang_row), apply_rope(x[..., h:], ang_col)], axis=-1)


def dwconv(u, w, b):
    y = lax.conv_general_dilated(u, w[:, None, :].astype(u.dtype), window_strides=(1,),
                                 padding=[(CONV_LEFT, CONV_W - 1 - CONV_LEFT)],
                                 dimension_numbers=('NWC', 'WIO', 'NWC'),
                                 feature_group_count=u.shape[-1])
    return y + b.astype(u.dtype)


def _linear_combine(e1, e2):
    a1, b1 = e1
    a2, b2 = e2
    return a1 * a2, a2 * b1 + b2


def lru_scan(u, wa, ba, wx, bx, lam, h0, reverse):
    B, L, _ = u.shape
    ub = u.reshape(B, L, LRU_HEADS, LRU_BW)
    r = jax.nn.sigmoid(jnp.einsum('blhi,hij->blhj', ub, wa).reshape(B, L, D_RNN) + ba)
    i = jax.nn.sigmoid(jnp.einsum('blhi,hij->blhj', ub, wx).reshape(B, L, D_RNN) + bx)
    log_a = -LRU_C * r * jax.nn.softplus(-lam)
    a = jnp.exp(log_a)
    b = jnp.sqrt(-jnp.expm1(2.0 * log_a)) * (i * u)
    a_cum, b_cum = lax.associative_scan(_linear_combine, (a, b), reverse=reverse, axis=1)
    return a_cum * h0[:, None, :] + b_cum


def lru_mixer(ax, ay, ax_c, ay_c, conv_w, conv_b, wa, ba, wx, bx, lam):
    f32 = jnp.float32
    wa, ba, wx, bx, lam = (t.astype(f32) for t in (wa, ba, wx, bx, lam))
    u = dwconv(ax, conv_w, conv_b).astype(f32)
    u_c = dwconv(ax_c, conv_w, conv_b).astype(f32)
    zero = jnp.zeros((ax.shape[0], D_RNN), f32)
    hc_f = lru_scan(u_c, wa[0], ba[0], wx[0], bx[0], lam[0], zero, False)
    hc_b = lru_scan(u_c, wa[1], ba[1], wx[1], bx[1], lam[1], zero, True)
    h_f = lru_scan(u, wa[0], ba[0], wx[0], bx[0], lam[0], hc_f[:, -1], False)
    h_b = lru_scan(u, wa[1], ba[1], wx[1], bx[1], lam[1], hc_b[:, 0], True)
    y = ((h_f + h_b) * jax.nn.gelu(ay.astype(f32))).astype(ax.dtype)
    if ay_c is None:
        return y, None
    y_c = ((hc_f + hc_b) * jax.nn.gelu(ay_c.astype(f32))).astype(ax.dtype)
    return y, y_c


def retention_chunked(q, k, v, log_g, s0):
    B, H, L, DK = k.shape
    DV = v.shape[-1]
    C = RET_CHUNK
    N = L // C
    pos = jnp.arange(C, dtype=jnp.float32)
    kc = k.reshape(B, H, N, C, DK)
    vc = v.reshape(B, H, N, C, DV)
    k_dec = jnp.exp(log_g[:, None] * (C - 1 - pos))
    chunk_kv = jnp.einsum('bhncd,bhnce->nbhde', kc * k_dec[None, :, None, :, None], vc)
    chunk_decay = jnp.exp(log_g * C)[None, :, None, None]

    def step(s, kv):
        return chunk_decay * s + kv, s

    s_final, s_before = lax.scan(step, s0, chunk_kv)
    if q is None:
        return None, s_final
    qc = q.reshape(B, H, N, C, DK)
    rel = pos[:, None] - pos[None, :]
    dmask = jnp.where(rel >= 0, jnp.exp(log_g[:, None, None] * jnp.maximum(rel, 0.0)), 0.0)
    att = jnp.einsum('bhnid,bhnjd->bhnij', qc, kc) * dmask[None, :, None]
    y = jnp.einsum('bhnij,bhnje->bhnie', att, vc)
    q_dec = jnp.exp(log_g[:, None] * (pos + 1.0))
    y = y + jnp.einsum('bhnid,nbhde->bhnie', qc * q_dec[None, :, None, :, None], s_before)
    return y.reshape(B, H, L, DV), s_final


def retention_out(y, g):
    mu = jnp.mean(y, axis=-1, keepdims=True)
    var = jnp.mean(jnp.square(y - mu), axis=-1, keepdims=True)
    yn = (y - mu) * lax.rsqrt(var + NORM_EPS)
    B, H, L, DV = y.shape
    yn = yn.transpose(0, 2, 1, 3).reshape(B, L, H * DV)
    return (jax.nn.silu(g.astype(jnp.float32)) * yn).astype(g.dtype)


def retention_mixer(bq, bk, bv, bg, bq_c, bk_c, bv_c, bg_c, ret_lam, ang_ret):
    f32 = jnp.float32
    B, L, _ = bk.shape
    LC = bk_c.shape[1]
    log_g = -jax.nn.softplus(-ret_lam.astype(f32))
    k_scale = RET_DK ** -0.5

    def to_heads(t, n, d):
        return t.astype(f32).reshape(B, n, RET_HEADS, d).transpose(0, 2, 1, 3)

    def rev(t):
        return None if t is None else jnp.flip(t, axis=2)

    q = to_heads(apply_rope(bq.reshape(B, L, RET_HEADS, RET_DK), ang_ret), L, RET_DK)
    k = to_heads(apply_rope(bk.reshape(B, L, RET_HEADS, RET_DK), ang_ret), L, RET_DK) * k_scale
    v = to_heads(bv, L, RET_DV)
    q_c = None if bq_c is None else to_heads(bq_c, LC, RET_DK)
    k_c = to_heads(bk_c, LC, RET_DK) * k_scale
    v_c = to_heads(bv_c, LC, RET_DV)
    zero = jnp.zeros((B, RET_HEADS, RET_DK, RET_DV), f32)
    yc_f, s_f = retention_chunked(q_c, k_c, v_c, log_g[0], zero)
    yc_b, s_b = retention_chunked(rev(q_c), rev(k_c), rev(v_c), log_g[1], zero)
    y_f, _ = retention_chunked(q, k, v, log_g[0], s_f)
    y_b, _ = retention_chunked(rev(q), rev(k), rev(v), log_g[1], s_b)
    y = retention_out(y_f + rev(y_b), bg)
    if q_c is None:
        return y, None
    return y, retention_out(yc_f + rev(yc_b), bg_c)


def attend_with_sink(q, ks, vs, masks, sink):
    scale = ATT_HD ** -0.5
    scores = []
    for k, m in zip(ks, masks):
        s = jnp.einsum('bqgrd,bkgd->bgrqk', q, k).astype(jnp.float32) * scale
        scores.append(s if m is None else jnp.where(m, s, -jnp.inf))
    sink_col = jnp.broadcast_to(sink[None, :, :, None, None], scores[0].shape[:-1] + (1,))
    p = jax.nn.softmax(jnp.concatenate(scores + [sink_col], axis=-1), axis=-1)
    out = jnp.zeros(q.shape, jnp.float32)
    off = 0
    for k, v in zip(ks, vs):
        n = k.shape[1]
        out = out + jnp.einsum('bgrqk,bkgd->bqgrd', p[..., off:off + n].astype(v.dtype), v).astype(jnp.float32)
        off += n
    return out.astype(q.dtype)


def window_attention(q, k, v, k_c, v_c, sink):
    B, S = q.shape[:2]
    n_blocks = S // ATT_BLOCK
    span = ATT_BLOCK + 2 * ATT_WIN
    pad = ((0, 0), (ATT_WIN, ATT_WIN), (0, 0), (0, 0))
    k_p = jnp.pad(k, pad)
    v_p = jnp.pad(v, pad)
    offs_q = jnp.arange(ATT_BLOCK)
    offs_k = jnp.arange(span) - ATT_WIN

    def block(bi):
        start = bi * ATT_BLOCK
        q_b = lax.dynamic_slice_in_dim(q, start, ATT_BLOCK, axis=1)
        k_b = lax.dynamic_slice_in_dim(k_p, start, span, axis=1)
        v_b = lax.dynamic_slice_in_dim(v_p, start, span, axis=1)
        q_pos = start + offs_q
        k_pos = start + offs_k
        mask = ((jnp.abs(q_pos[:, None] - k_pos[None, :]) <= ATT_WIN)
                & (k_pos >= 0)[None, :] & (k_pos < S)[None, :])
        return attend_with_sink(q_b, (k_b, k_c), (v_b, v_c), (mask, None), sink)

    out = lax.map(block, jnp.arange(n_blocks))
    return out.transpose(1, 0, 2, 3, 4, 5).reshape(B, S, ATT_HEADS * ATT_HD)


def attention_mixer(cq, ck, cv, cq_c, ck_c, cv_c, sink, ang_row, ang_col):
    B, L, _ = cq.shape
    LC = ck_c.shape[1]
    G, R, HD = ATT_KV_HEADS, ATT_HEADS // ATT_KV_HEADS, ATT_HD
    sink = sink.astype(jnp.float32).reshape(G, R)
    q = axial_rope(cq.reshape(B, L, G, R, HD), ang_row, ang_col)
    k = axial_rope(ck.reshape(B, L, G, HD), ang_row, ang_col)
    v = cv.reshape(B, L, G, HD)
    k_c = ck_c.reshape(B, LC, G, HD)
    v_c = cv_c.reshape(B, LC, G, HD)
    y = window_attention(q, k, v, k_c, v_c, sink)
    if cq_c is None:
        return y, None
    y_c = attend_with_sink(cq_c.reshape(B, LC, G, R, HD), (k_c,), (v_c,), (None,), sink)
    return y, y_c.reshape(B, LC, ATT_HEADS * HD)


def merge(ya, yb, yc, gates, w_ba, w_bb, w_bc, w_o):
    g_a, g_b, g_c = jnp.split(jax.nn.sigmoid(gates), N_BRANCHES, axis=-1)
    return (g_a * (ya @ w_ba) + g_b * (yb @ w_bb) + g_c * (yc @ w_bc)) @ w_o


def mixer_sublayer(u, u_c, w_in, conv_w, conv_b, wa, ba, wx, bx, lam, ret_lam, sink,
                   w_ba, w_bb, w_bc, w_o, ang_ret, ang_row, ang_col, with_ctx_out):
    lat = split_cols(u @ w_in)
    if with_ctx_out:
        ctxp = split_cols(u_c @ w_in)
    else:
        w_parts = split_cols(w_in)
        ctxp = [u_c @ w_parts[i] if i in CTX_STATE_PARTS else None for i in range(len(IN_SPLITS))]
    ax, ay, bq, bk, bv, bg, cq, ck, cv, gates = lat
    ax_c, ay_c, bq_c, bk_c, bv_c, bg_c, cq_c, ck_c, cv_c, gates_c = ctxp
    ya, ya_c = lru_mixer(ax, ay, ax_c, ay_c, conv_w, conv_b, wa, ba, wx, bx, lam)
    yb, yb_c = retention_mixer(bq, bk, bv, bg, bq_c, bk_c, bv_c, bg_c, ret_lam, ang_ret)
    yc, yc_c = attention_mixer(cq, ck, cv, cq_c, ck_c, cv_c, sink, ang_row, ang_col)
    out = merge(ya, yb, yc, gates, w_ba, w_bb, w_bc, w_o)
    if not with_ctx_out:
        return out, None
    return out, merge(ya_c, yb_c, yc_c, gates_c, w_ba, w_bb, w_bc, w_o)


def hier_moe(t, w_gr, b_gr, w_er, b_er, w_gate, w_up, w_down):
    N, D = t.shape
    f32 = jnp.float32
    tf = t.astype(f32)
    g_logit = tf @ w_gr.astype(f32) + b_gr.astype(f32)
    g_idx = jnp.argmax(g_logit, axis=-1)
    g_w = jnp.take_along_axis(jax.nn.softmax(g_logit, axis=-1), g_idx[:, None], axis=-1)
    e_logit = (tf @ w_er.astype(f32) + b_er.astype(f32)).reshape(N, N_GROUPS, EXPERTS_PER_GROUP)
    e_logit = jnp.take_along_axis(e_logit, g_idx[:, None, None], axis=1)[:, 0]
    top_p, top_i = lax.top_k(jax.nn.softmax(e_logit, axis=-1), TOP_K)
    top_w = top_p / jnp.sum(top_p, axis=-1, keepdims=True) * g_w
    A = N * TOP_K
    eid = (g_idx[:, None] * EXPERTS_PER_GROUP + top_i).reshape(-1).astype(jnp.int32)
    tok = jnp.repeat(jnp.arange(N, dtype=jnp.int32), TOP_K)
    order = jnp.argsort(eid)
    s_e, s_t, s_w = eid[order], tok[order], top_w.reshape(-1)[order]
    counts = jax.ops.segment_sum(jnp.ones_like(eid), eid, num_segments=N_EXPERTS)
    padded = (counts + MOE_BLOCK - 1) // MOE_BLOCK * MOE_BLOCK
    p_end = jnp.cumsum(padded)
    p_start = p_end - padded
    u_start = jnp.cumsum(counts) - counts
    dest = p_start[s_e] + jnp.arange(A, dtype=jnp.int32) - u_start[s_e]
    P = (A + N_EXPERTS * (MOE_BLOCK - 1) + MOE_BLOCK - 1) // MOE_BLOCK * MOE_BLOCK
    n_blk = P // MOE_BLOCK
    row_tok = jnp.full((P,), N, jnp.int32).at[dest].set(s_t)
    row_w = jnp.zeros((P,), f32).at[dest].set(s_w)
    block_e = jnp.minimum(jnp.searchsorted(p_end, jnp.arange(n_blk, dtype=jnp.int32) * MOE_BLOCK,
                                           side='right'), N_EXPERTS - 1)
    t_pad = jnp.concatenate([t, jnp.zeros((1, D), t.dtype)], axis=0)

    def run(bi):
        rows = lax.dynamic_slice_in_dim(row_tok, bi * MOE_BLOCK, MOE_BLOCK)
        xb = t_pad[rows]
        e = block_e[bi]
        return (jax.nn.silu(xb @ w_gate[e]) * (xb @ w_up[e])) @ w_down[e]

    y_rows = lax.map(run, jnp.arange(n_blk)).reshape(P, D)
    y = jax.ops.segment_sum(y_rows * row_w[:, None].astype(y_rows.dtype), row_tok, num_segments=N + 1)
    return y[:N]


def setup_inputs(seed: int = 0) -> dict:
    key = jax.random.key(seed)
    ks = jax.random.split(key, 30)
    f32 = jnp.float32

    def nrm(i, shape, scale):
        return jax.random.normal(ks[i], shape, f32) * scale

    D = D_MODEL
    a0 = jax.random.uniform(ks[15], (DEPTH, 2, D_RNN), f32, 0.9, 0.999)
    a_base = a0 ** (1.0 / LRU_C)
    gamma = 1.0 - 2.0 ** (-5.0 - jnp.arange(RET_HEADS, dtype=f32))
    return {
        'x': nrm(0, (BATCH, SEQ, D), 1.0),
        'c': nrm(1, (BATCH, D), 1.0),
        'ctx': nrm(2, (BATCH, CTX_LEN, D), 1.0),
        'c_ctx': nrm(3, (D,), 1.0),
        'w_mod': nrm(4, (DEPTH, D, N_MOD * D), 0.5 * D ** -0.5),
        'b_mod': nrm(5, (DEPTH, N_MOD * D), 0.02),
        'norm1_g': 1.0 + nrm(6, (DEPTH, D), 0.02),
        'norm2_g': 1.0 + nrm(7, (DEPTH, D), 0.02),
        'w_in': nrm(8, (DEPTH, D, D_IN), D ** -0.5),
        'lru_conv_w': nrm(9, (DEPTH, CONV_W, D_RNN), CONV_W ** -0.5),
        'lru_conv_b': nrm(10, (DEPTH, D_RNN), 0.02),
        'lru_wa': nrm(11, (DEPTH, 2, LRU_HEADS, LRU_BW, LRU_BW), LRU_BW ** -0.5),
        'lru_ba': nrm(12, (DEPTH, 2, D_RNN), 0.02),
        'lru_wx': nrm(13, (DEPTH, 2, LRU_HEADS, LRU_BW, LRU_BW), LRU_BW ** -0.5),
        'lru_bx': nrm(14, (DEPTH, 2, D_RNN), 0.02),
        'lru_lambda': jnp.log(a_base) - jnp.log1p(-a_base),
        'ret_lambda': jnp.log(gamma) - jnp.log1p(-gamma) + nrm(16, (DEPTH, 2, RET_HEADS), 0.05),
        'attn_sink': nrm(17, (DEPTH, ATT_HEADS), 0.5),
        'w_branch_a': nrm(18, (DEPTH, D_RNN, D), D_RNN ** -0.5),
        'w_branch_b': nrm(19, (DEPTH, RET_HEADS * RET_DV, D), (RET_HEADS * RET_DV) ** -0.5),
        'w_branch_c': nrm(20, (DEPTH, ATT_HEADS * ATT_HD, D), (ATT_HEADS * ATT_HD) ** -0.5),
        'w_out': nrm(21, (DEPTH, D, D), D ** -0.5),
        'router_group_w': nrm(22, (DEPTH, D, N_GROUPS), D ** -0.5),
        'router_group_b': nrm(23, (DEPTH, N_GROUPS), 0.01),
        'router_expert_w': nrm(24, (DEPTH, D, N_EXPERTS), D ** -0.5),
        'router_expert_b': nrm(25, (DEPTH, N_EXPERTS), 0.01),
        'expert_w_gate': nrm(26, (DEPTH, N_EXPERTS, D, D_EXPERT), D ** -0.5),
        'expert_w_up': nrm(27, (DEPTH, N_EXPERTS, D, D_EXPERT), D ** -0.5),
        'expert_w_down': nrm(28, (DEPTH, N_EXPERTS, D_EXPERT, D), D_EXPERT ** -0.5),
        'final_norm_g': 1.0 + nrm(29, (D,), 0.02),
    }


def reference(x, c, ctx, c_ctx, w_mod, b_mod, norm1_g, norm2_g, w_in, lru_conv_w, lru_conv_b,
              lru_wa, lru_ba, lru_wx, lru_bx, lru_lambda, ret_lambda, attn_sink, w_branch_a,
              w_branch_b, w_branch_c, w_out, router_group_w, router_group_b, router_expert_w,
              router_expert_b, expert_w_gate, expert_w_up, expert_w_down, final_norm_g):
    B, S, D = x.shape
    LC = ctx.shape[1]
    rows = S // GRID_W
    row = jnp.broadcast_to(jnp.arange(rows)[:, None], (rows, GRID_W)).reshape(-1).astype(jnp.float32)
    col = jnp.broadcast_to(jnp.arange(GRID_W)[None, :], (rows, GRID_W)).reshape(-1).astype(jnp.float32)
    ang_row = rope_angles(row, ATT_HD // 2)
    ang_col = rope_angles(col, ATT_HD // 2)
    ang_ret = rope_angles(jnp.arange(S, dtype=jnp.float32), RET_DK)
    s_c = jax.nn.silu(c)
    s_cc = jax.nn.silu(c_ctx)
    h, hc = x, ctx
    for l in range(DEPTH):
        last = l == DEPTH - 1
        mod = jnp.split((s_c @ w_mod[l] + b_mod[l])[:, None, :], N_MOD, axis=-1)
        mod_c = jnp.split(s_cc @ w_mod[l] + b_mod[l], N_MOD, axis=-1)
        u = modulate(rmsnorm(h, norm1_g[l]), mod[0], mod[1])
        u_c = modulate(rmsnorm(hc, norm1_g[l]), mod_c[0], mod_c[1])
        out, out_c = mixer_sublayer(u, u_c, w_in[l], lru_conv_w[l], lru_conv_b[l], lru_wa[l], lru_ba[l],
                                    lru_wx[l], lru_bx[l], lru_lambda[l], ret_lambda[l], attn_sink[l],
                                    w_branch_a[l], w_branch_b[l], w_branch_c[l], w_out[l],
                                    ang_ret, ang_row, ang_col, not last)
        h = h + mod[2] * out
        v = modulate(rmsnorm(h, norm2_g[l]), mod[3], mod[4]).reshape(B * S, D)
        moe_w = (router_group_w[l], router_group_b[l], router_expert_w[l], router_expert_b[l],
                 expert_w_gate[l], expert_w_up[l], expert_w_down[l])
        if last:
            h = h + mod[5] * hier_moe(v, *moe_w).reshape(B, S, D)
        else:
            hc = hc + mod_c[2] * out_c
            v_c = modulate(rmsnorm(hc, norm2_g[l]), mod_c[3], mod_c[4]).reshape(B * LC, D)
            y = hier_moe(jnp.concatenate([v, v_c], axis=0), *moe_w)
            h = h + mod[5] * y[:B * S].reshape(B, S, D)
            hc = hc + mod_c[5] * y[B * S:].reshape(B, LC, D)
    return rmsnorm(h, final_norm_g)
```

```python
import numpy as np
from contextlib import ExitStack, contextmanager
import concourse.bass as bass
import concourse.mybir as mybir
from concourse.bass_utils import run_bass_kernel_spmd

F32 = mybir.dt.float32
BF16 = mybir.dt.bfloat16
I32 = mybir.dt.int32
AF = mybir.ActivationFunctionType
ALU = mybir.AluOpType
AX = mybir.AxisListType

DEPTH = 4
T = 4352
NT = 34
LC = 256
SEQ = 4096
TB = [(0, 256)] + [(256 + 512 * i, 512) for i in range(8)]
CAP = 1024
NSLOT = 32 * CAP
EPS = 1e-6
N_CORES = 8


class SC:
    def __init__(self, nc, name):
        self.sem = nc.alloc_semaphore(name)
        self.n = 0


class Res:
    def __init__(self, name=""):
        self.w = None
        self.rs = {}
        self.dsc = None
        self.name = name


class Tile(Res):
    def __init__(self, t, name):
        super().__init__(name)
        self.t = t

    def __getitem__(self, k):
        return self.t[k]


class Eng:
    def __init__(self, nc, e, name, selfsync=True):
        self.e = e
        self.sc = SC(nc, "eng_" + name)
        self.waited = {}
        self.selfsync = selfsync

    def wait(self, tok):
        sc, val = tok
        if sc is self.sc and not self.selfsync:
            return
        if self.waited.get(sc, 0) >= val:
            return
        self.waited[sc] = val
        self.e.wait_ge(sc.sem, val)


class Kern:
    def __init__(self, nc):
        self.nc = nc
        self.pe = Eng(nc, nc.tensor, "pe", selfsync=False)
        self.act = Eng(nc, nc.scalar, "act")
        self.dve = Eng(nc, nc.vector, "dve")
        self.pool = Eng(nc, nc.gpsimd, "pool")
        self.sp = Eng(nc, nc.sync, "sp")
        self.engs = [self.pe, self.act, self.dve, self.pool, self.sp]
        self.scs = [e.sc for e in self.engs]
        self.free_dsc = []
        self.ndsc = 0
        self.stacks = []
        self.uid = 0
        self.bank_i = 0

    @contextmanager
    def phase(self):
        st = ExitStack()
        self.stacks.append((st, []))
        try:
            yield
        finally:
            self.barrier()
            _, used = self.stacks.pop()
            for r in used:
                if r.dsc is not None:
                    self.free_dsc.append(r.dsc)
                    r.dsc = None
            st.close()

    def sb(self, name, shape, dt):
        self.uid += 1
        t = self.stacks[-1][0].enter_context(self.nc.sbuf_tensor(f"{name}_{self.uid}", list(shape), dt))
        r = Tile(t, name)
        self.stacks[-1][1].append(r)
        return r

    def ps(self, name, shape, dt):
        self.uid += 1
        t = self.stacks[-1][0].enter_context(self.nc.psum_tensor(f"{name}_{self.uid}", list(shape), dt))
        r = Tile(t, name)
        self.stacks[-1][1].append(r)
        return r

    def res(self, name=""):
        r = Res(name)
        self.stacks[-1][1].append(r)
        return r

    def get_dsc(self, r):
        if r.dsc is None:
            if self.free_dsc:
                r.dsc = self.free_dsc.pop()
            else:
                self.ndsc += 1
                r.dsc = SC(self.nc, f"dma{self.ndsc}")
                self.scs.append(r.dsc)
        return r.dsc

    def _pre(self, eng, reads, writes):
        for r in reads:
            if r.w is not None:
                eng.wait(r.w)
        for w in writes:
            if w.w is not None:
                eng.wait(w.w)
            for sc, val in w.rs.items():
                eng.wait((sc, val))

    def _post(self, tok, reads, writes):
        for r in reads:
            if r.rs.get(tok[0], 0) < tok[1]:
                r.rs[tok[0]] = tok[1]
        for w in writes:
            w.w = tok
            w.rs = {}

    def op(self, eng, fn, r=(), w=(), inc=True):
        self._pre(eng, r, w)
        ins = fn()
        if inc:
            eng.sc.n += 1
            ins.then_inc(eng.sc.sem, 1)
            tok = (eng.sc, eng.sc.n)
        else:
            tok = (eng.sc, eng.sc.n + 1)
        self._post(tok, r, w)
        return ins

    def V(self, fn, r=(), w=()):
        return self.op(self.dve, fn, r, w)

    def A(self, fn, r=(), w=()):
        return self.op(self.act, fn, r, w)

    def G(self, fn, r=(), w=()):
        return self.op(self.pool, fn, r, w)

    def P(self, fn, r=(), w=(), inc=True):
        return self.op(self.pe, fn, r, w, inc)

    def dma(self, q, out, in_, r=(), w=(), dres=None, **kw):
        self._pre(q, r, w)
        ins = q.e.dma_start(out=out, in_=in_, **kw)
        sc = self.get_dsc(dres)
        sc.n += 16
        ins.then_inc(sc.sem, 16)
        self._post((sc, sc.n), r, w)
        return ins

    def idma(self, out, in_, out_off, in_off, r=(), w=(), dres=None):
        q = self.pool
        self._pre(q, r, w)
        ins = q.e.indirect_dma_start(out=out, out_offset=out_off, in_=in_, in_offset=in_off)
        sc = self.get_dsc(dres)
        sc.n += 16
        ins.then_inc(sc.sem, 16)
        self._post((sc, sc.n), r, w)
        return ins

    def barrier(self):
        for e in self.engs:
            for sc in self.scs:
                if sc.n > 0:
                    e.wait((sc, sc.n))

    def bank(self):
        self.bank_i = (self.bank_i + 1) % 8
        return self.bank_i


def bc(ap, shape):
    return ap.to_broadcast(list(shape))


class _Stop(Exception):
    pass


def build_program(n_layers=DEPTH, dbg=None, stop=None):
    scr = {}

    def chk(p):
        if stop == p:
            if dbg is not None and dbg[0] in scr:
                scr["K"].barrier()
                scr["K"].dma(scr["K"].sp, scr["DBG"], scr[dbg[0]], dres=scr["cst"])
                scr["K"].barrier()
            raise _Stop()
    nc = bass.Bass("TRN2", target_bir_lowering=False)
    K = Kern(nc)

    def din(name, shape, dt=F32):
        return nc.dram_tensor(name, list(shape), dt, kind="ExternalInput").ap()

    def dscr(name, shape, dt):
        return nc.dram_tensor(name, list(shape), dt, kind="Internal").ap()

    D = {}
    D["h0T"] = din("h0T", [1024, T])
    D["cT"] = din("cT", [128, 8, 2])
    D["w_mod"] = din("w_mod", [DEPTH, 1024, 6144])
    D["b_modT"] = din("b_modT", [128, DEPTH, 48])
    D["g1T"] = din("g1T", [128, DEPTH, 8])
    D["g2T"] = din("g2T", [128, DEPTH, 8])
    D["gfT"] = din("gfT", [128, 8])
    D["w_in"] = din("w_in", [DEPTH, 1024, 9728])
    D["conv_wT"] = din("conv_wT", [128, DEPTH, 8, 4])
    D["conv_bT"] = din("conv_bT", [128, DEPTH, 8])
    D["wa_bd"] = din("wa_bd", [DEPTH, 8, 128, 2, 128])
    D["wx_bd"] = din("wx_bd", [DEPTH, 8, 128, 2, 128])
    D["lru_baT"] = din("lru_baT", [128, DEPTH, 2, 8])
    D["lru_bxT"] = din("lru_bxT", [128, DEPTH, 2, 8])
    D["lru_lamT"] = din("lru_lamT", [128, DEPTH, 2, 8])
    D["ret_lam_rep"] = din("ret_lam_rep", [128, DEPTH, 2, 8])
    D["ret_lam_S"] = din("ret_lam_S", [128, DEPTH, 2, 4])
    D["sink_rep"] = din("sink_rep", [128, DEPTH, 16])
    D["w_ba"] = din("w_ba", [DEPTH, 1024, 1024])
    D["w_bb"] = din("w_bb", [DEPTH, 1024, 1024])
    D["w_bc"] = din("w_bc", [DEPTH, 1024, 1024])
    D["w_out"] = din("w_out", [DEPTH, 1024, 1024])
    D["w_rt"] = din("w_rt", [128, DEPTH, 8, 36])
    D["b_rt"] = din("b_rt", [128, DEPTH, 36])
    D["e_wg"] = din("e_wg", [DEPTH, 32, 1024, 512])
    D["e_wu"] = din("e_wu", [DEPTH, 32, 1024, 512])
    D["e_wd"] = din("e_wd", [DEPTH, 32, 512, 1024])
    D["consts"] = din("consts", [128, 1412])
    D["rope"] = din("rope", [NT, 128, 256])

    OUT = nc.dram_tensor("outT", [1024, SEQ], F32, kind="ExternalOutput").ap()
    DBG = None
    if dbg is not None:
        DBG = nc.dram_tensor("dbg", list(dbg[1]), dbg[2], kind="ExternalOutput").ap()

    hT = dscr("hT", [1024, T], F32)
    yaT = dscr("yaT", [1024, T], BF16)
    ybT = dscr("ybT", [1024, T], BF16)
    ycT = dscr("ycT", [1024, T], BF16)
    sgT = dscr("sgT", [3072, T], BF16)
    rqT = dscr("rqT", [512, T], BF16)
    rkT = dscr("rkT", [512, T], BF16)
    rk = dscr("rk", [T, 512], BF16)
    rv = dscr("rv", [T, 1024], BF16)
    rg = dscr("rg", [T, 1024], BF16)
    aqT = dscr("aqT", [1024, T], BF16)
    akT = dscr("akT", [256, T], BF16)
    av = dscr("av", [T, 256], BF16)
    SfD = dscr("SfD", [NT, 128, 512], BF16)
    xslot = dscr("xslot", [NSLOT, 1024], BF16)
    yslot = dscr("yslot", [NSLOT, 1024], F32)

    scr.update(dict(K=K, DBG=DBG, hT=hT, yaT=yaT, ybT=ybT, ycT=ycT, sgT=sgT, rqT=rqT, rkT=rkT, rk=rk, rv=rv, rg=rg,
                    aqT=aqT, akT=akT, av=av, xslot=xslot, yslot=yslot))

    def fm(ap2d, t0, bs):
        return ap2d.rearrange("(k p) t -> p k t", p=128)[:, :, t0:t0 + bs]

    with K.phase():
        cst = K.sb("cst", [128, 1412], F32)
        scr["cst"] = cst
        K.dma(K.sp, cst[:], D["consts"], w=[cst], dres=cst)
        C_ID, C_R1, C_M1, C_R2, C_M2, C_I2, C_MP, C_MN = [i * 128 for i in range(8)]
        C_CV = 1024
        C_TRI = 1028
        C_EB = 1156
        C_ONE = 1188
        identb = K.sb("identb", [128, 128], BF16)
        identf = cst
        onesb = K.sb("onesb", [128, 128], BF16)
        maskp = K.sb("maskp", [128, 128], BF16)
        maskn = K.sb("maskn", [128, 128], BF16)
        trib = K.sb("trib", [128, 128], BF16)
        epsT = K.sb("epsT", [128, 1], F32)
        K.V(lambda: nc.vector.tensor_copy(out=identb[:], in_=cst[:, C_ID:C_ID + 128]), r=[cst], w=[identb])
        K.V(lambda: nc.vector.tensor_copy(out=onesb[:], in_=cst[:, C_ONE:C_ONE + 128]), r=[cst], w=[onesb])
        K.V(lambda: nc.vector.tensor_copy(out=maskp[:], in_=cst[:, C_MP:C_MP + 128]), r=[cst], w=[maskp])
        K.V(lambda: nc.vector.tensor_copy(out=maskn[:], in_=cst[:, C_MN:C_MN + 128]), r=[cst], w=[maskn])
        K.V(lambda: nc.vector.tensor_copy(out=trib[:], in_=cst[:, C_TRI:C_TRI + 128]), r=[cst], w=[trib])
        K.V(lambda: nc.vector.memset(epsT[:], EPS), w=[epsT])
        modT = K.sb("modT", [128, DEPTH, 48, 2], F32)
        g1T = K.sb("g1T", [128, DEPTH, 8], F32)
        g2T = K.sb("g2T", [128, DEPTH, 8], F32)
        gfT = K.sb("gfT", [128, 8], F32)
        K.dma(K.sp, g1T[:], D["g1T"], w=[g1T], dres=g1T)
        K.dma(K.sp, g2T[:], D["g2T"], w=[g2T], dres=g2T)
        K.dma(K.sp, gfT[:], D["gfT"], w=[gfT], dres=gfT)
        gs1 = K.sb("gs1", [128, 8, 2], F32)
        gs2 = K.sb("gs2", [128, 8, 2], F32)
        slots = K.sb("slots", [128, NT, 2], I32)
        wts = K.sb("wts", [128, NT, 2], F32)

        def modv(l, m, v):
            return modT[:, l, m * 8:(m + 1) * 8, v]

        def mods(l, m, k, v):
            return modT[:, l, m * 8 + k, v:v + 1]

        sT = K.sb("sT", [128, 8, 2], F32)
        bm = K.sb("bm", [128, DEPTH, 48], F32)

        def mod_chunk(lm, jj, wt, pm):
            K.dma(K.sp, wt[:], D["w_mod"][lm, :, jj * 512:(jj + 1) * 512].rearrange("(k p) n -> p k n", p=128), w=[wt], dres=wt)
            for j4 in range(4):
                j = jj * 4 + j4
                for k in range(8):
                    K.P(lambda: nc.tensor.matmul(pm[:, j, :], lhsT=wt[:, k, j4 * 128:(j4 + 1) * 128], rhs=sT[:, k, :],
                                                 start=(k == 0), stop=(k == 7)), r=[wt, sT], w=[pm], inc=(k == 7))

        def mod_chunk_small(lm, j, wt, pm):
            K.dma(K.pool, wt[:], D["w_mod"][lm, :, j * 128:(j + 1) * 128].rearrange("(k p) n -> p k n", p=128), w=[wt], dres=wt)
            for k in range(8):
                K.P(lambda: nc.tensor.matmul(pm[:, j, :], lhsT=wt[:, k, :], rhs=sT[:, k, :],
                                             start=(k == 0), stop=(k == 7)), r=[wt, sT], w=[pm], inc=(k == 7))

        def mod_finish(lm, pm):
            K.V(lambda: nc.vector.tensor_tensor(out=modT[:, lm], in0=pm[:], in1=bc(bm[:, lm, :].unsqueeze(2), [128, 48, 2]),
                                                op=ALU.add), r=[pm, bm], w=[modT])

        with K.phase():
            zt = K.sb("zt", [128, 8192], BF16)
            K.G(lambda: nc.gpsimd.memset(zt[:], 0.0), w=[zt])
            xz = xslot.rearrange("(p a) f -> p (a f)", p=128)
            for zi in range(NSLOT * 1024 // (128 * 8192)):
                K.dma(K.sp, xz[:, zi * 8192:(zi + 1) * 8192], zt[:], r=[zt], dres=zt)
            cT = K.sb("cT", [128, 8, 2], F32)
            sgm = K.sb("sgm", [128, 8, 2], F32)
            K.dma(K.sp, cT[:], D["cT"], w=[cT], dres=cT)
            K.dma(K.sp, bm[:], D["b_modT"], w=[bm], dres=bm)
            K.A(lambda: nc.scalar.activation(out=sgm[:], in_=cT[:], func=AF.Sigmoid), r=[cT], w=[sgm])
            K.V(lambda: nc.vector.tensor_tensor(out=sT[:], in0=cT[:], in1=sgm[:], op=ALU.mult), r=[cT, sgm], w=[sT])
            wm = [K.sb(f"wm{i}", [128, 8, 512], F32) for i in range(6)]
            pm = K.ps("pm", [128, 48, 2], F32)
            if n_layers > 0:
                for jj in range(12):
                    mod_chunk(0, jj, wm[jj % 6], pm)
                mod_finish(0, pm)

        def norm_block(hb, bs, sqb, tmp, rt, rstd, pbank, pres):
            K.A(lambda: nc.scalar.activation(out=sqb[:, :, :bs], in_=hb[:, :, :bs], func=AF.Square), r=[hb], w=[sqb])
            for k in range(8):
                K.P(lambda: nc.tensor.matmul(pbank[:, :bs], lhsT=onesb[:], rhs=sqb[:, k, :bs], start=(k == 0), stop=(k == 7)),
                    r=[onesb, sqb], w=[pres], inc=(k == 7))
            K.A(lambda: nc.scalar.activation(out=rt[:, :bs], in_=pbank[:, :bs], func=AF.Sqrt, bias=epsT[:, 0:1], scale=1.0 / 1024.0),
                r=[pres, epsT], w=[rt])
            K.V(lambda: nc.vector.reciprocal(out=rstd[:, :bs], in_=rt[:, :bs]), r=[rt], w=[rstd])
            K.V(lambda: nc.vector.tensor_tensor(out=tmp[:, :, :bs], in0=hb[:, :, :bs],
                                                in1=bc(rstd[:, :bs].unsqueeze(1), [128, 8, bs]), op=ALU.mult),
                r=[hb, rstd], w=[tmp])

        def transposes(src_ap_fn, n, pst, pst_res, srcs):
            for i in range(n):
                K.P(lambda: nc.tensor.transpose(out=pst[:, i, :], in_=src_ap_fn(i), identity=identb[:]),
                    r=list(srcs) + [identb], w=[pst_res], inc=(i == n - 1))

        for l in range(n_layers):
          try:
            src_h = D["h0T"] if l == 0 else hT
            for v in range(2):
                K.V(lambda: nc.vector.scalar_tensor_tensor(out=gs1[:, :, v], in0=modv(l, 1, v), scalar=1.0, in1=g1T[:, l, :],
                                                           op0=ALU.add, op1=ALU.mult), r=[modT, g1T], w=[gs1])
                K.V(lambda: nc.vector.scalar_tensor_tensor(out=gs2[:, :, v], in0=modv(l, 4, v), scalar=1.0, in1=g2T[:, l, :],
                                                           op0=ALU.add, op1=ALU.mult), r=[modT, g2T], w=[gs2])

            with K.phase():
                uT = K.sb("uT", [128, 8, T], BF16)
                with K.phase():
                    hbs = [K.sb(f"hb{i}", [128, 8, 512], F32) for i in range(2)]
                    sqb = K.sb("sqb", [128, 8, 512], BF16)
                    tmp = K.sb("tmp", [128, 8, 512], F32)
                    rt = K.sb("rt", [128, 512], F32)
                    rstd = K.sb("rstd", [128, 512], F32)
                    pb = K.ps("pb", [128, 2, 512], F32)
                    pres = [K.res(), K.res()]
                    for bi, (t0, bs) in enumerate(TB):
                        hb = hbs[bi % 2]
                        v = 1 if bi == 0 else 0
                        K.dma(K.sp, hb[:, :, :bs], fm(src_h, t0, bs), w=[hb], dres=hb)
                        norm_block(hb, bs, sqb, tmp, rt, rstd, pb[:, bi % 2, :], pres[bi % 2])
                        for k in range(8):
                            K.A(lambda: nc.scalar.activation(out=uT[:, k, t0:t0 + bs], in_=tmp[:, k, :bs], func=AF.Identity,
                                                             bias=mods(l, 0, k, v), scale=gs1[:, k, v:v + 1]),
                                r=[tmp, gs1, modT], w=[uT])
                        if l == 0:
                            K.dma(K.sp, fm(hT, t0, bs), hb[:, :, :bs], r=[hb], dres=hb)
                if dbg is not None and dbg[0] == "uT" and l == dbg[3]:
                    K.dma(K.sp, DBG.rearrange("(k p) t -> p k t", p=128), uT[:], r=[uT], dres=uT)
                chk("P1")

                with K.phase():
                    lam = K.sb("lam", [128, 2, 8], F32)
                    ex = K.sb("ex", [128, 2, 8], F32)
                    sp1 = K.sb("sp1", [128, 2, 8], F32)
                    sp2 = K.sb("sp2", [128, 2, 8], F32)
                    ba = K.sb("ba", [128, 2, 8], F32)
                    bx = K.sb("bx", [128, 2, 8], F32)
                    cw = K.sb("cw", [128, 8, 4], F32)
                    cb = K.sb("cb", [128, 8], F32)
                    K.dma(K.sp, lam[:], D["lru_lamT"][:, l], w=[lam], dres=lam)
                    K.dma(K.sp, ba[:], D["lru_baT"][:, l], w=[ba], dres=ba)
                    K.dma(K.sp, bx[:], D["lru_bxT"][:, l], w=[bx], dres=bx)
                    K.dma(K.sp, cw[:], D["conv_wT"][:, l], w=[cw], dres=cw)
                    K.dma(K.sp, cb[:], D["conv_bT"][:, l], w=[cb], dres=cb)
                    K.A(lambda: nc.scalar.activation(out=ex[:], in_=lam[:], func=AF.Exp, scale=-1.0), r=[lam], w=[ex])
                    K.A(lambda: nc.scalar.activation(out=ex[:], in_=ex[:], func=AF.Ln, bias=1.0), r=[ex], w=[ex])
                    K.V(lambda: nc.vector.tensor_scalar(out=sp1[:], in0=ex[:], scalar1=-8.0, scalar2=None, op0=ALU.mult), r=[ex], w=[sp1])
                    K.V(lambda: nc.vector.tensor_scalar(out=sp2[:], in0=ex[:], scalar1=-16.0, scalar2=None, op0=ALU.mult), r=[ex], w=[sp2])
                    xa = K.sb("xa", [128, T + 8], F32)
                    uu = K.sb("uu", [128, T], F32)
                    ubs = [K.sb(f"ub{i}", [128, T], BF16) for i in range(2)]
                    hs = K.sb("hs", [128, T], F32)
                    rr_l = [K.sb(f"rr{i}", [128, 2048], F32) for i in range(2)]
                    ii_l = [K.sb(f"ii{i}", [128, 2048], F32) for i in range(2)]
                    a2_l = [K.sb(f"a2{i}", [128, 2048], F32) for i in range(2)]
                    sgi_ = [0]
                    stt = K.sb("stt", [128, 1], F32)
                    wxs = [K.sb(f"wxs{i}", [128, 8, 128], BF16) for i in range(2)]
                    wys = [K.sb(f"wys{i}", [128, 8, 128], BF16) for i in range(2)]
                    bdAs = [K.sb(f"bdA{i}", [128, 2, 128], BF16) for i in range(2)]
                    bdXs = [K.sb(f"bdX{i}", [128, 2, 128], BF16) for i in range(2)]
                    gy = K.sb("gy", [128, 512], BF16)
                    yos = [K.sb(f"yo{i}", [128, 512], BF16) for i in range(2)]
                    pb = K.ps("pb", [128, 8, 512], F32)
                    pres = [K.res() for _ in range(8)]
                    K.G(lambda: nc.gpsimd.memset(xa[:], 0.0), w=[xa])
                    win = D["w_in"][l].rearrange("(k p) n -> p k n", p=128)

                    def xoff(t):
                        return t + 2 if t < 256 else t + 5
                    yoi = [0]

                    def startup(c):
                        wx_, wy_, bdA, bdX, ub = wxs[c % 2], wys[c % 2], bdAs[c % 2], bdXs[c % 2], ubs[c % 2]
                        K.dma(K.pool, wx_[:], win[:, :, c * 128:(c + 1) * 128], w=[wx_], dres=wx_)
                        K.dma(K.pool, wy_[:], win[:, :, 1024 + c * 128:1024 + (c + 1) * 128], w=[wy_], dres=wy_)
                        K.dma(K.pool, bdA[:], D["wa_bd"][l, c], w=[bdA], dres=bdA)
                        K.dma(K.pool, bdX[:], D["wx_bd"][l, c], w=[bdX], dres=bdX)
                        for bi, (t0, bs) in enumerate(TB):
                            b = K.bank()
                            for k in range(8):
                                K.P(lambda: nc.tensor.matmul(pb[:, b, :bs], lhsT=wx_[:, k, :], rhs=uT[:, k, t0:t0 + bs],
                                                             start=(k == 0), stop=(k == 7)), r=[wx_, uT], w=[pres[b]], inc=(k == 7))
                            K.A(lambda: nc.scalar.copy(out=xa[:, xoff(t0):xoff(t0) + bs], in_=pb[:, b, :bs]), r=[pres[b]], w=[xa])
                        for (s0, n, po) in [(0, 256, 0), (256, 4096, 259)]:
                            K.V(lambda: nc.vector.tensor_scalar(out=uu[:, s0:s0 + n], in0=xa[:, po:po + n], scalar1=cw[:, c, 0:1],
                                                                scalar2=cb[:, c:c + 1], op0=ALU.mult, op1=ALU.add),
                                r=[xa, cw, cb], w=[uu])
                            for j in range(1, 3):
                                K.V(lambda: nc.vector.scalar_tensor_tensor(out=uu[:, s0:s0 + n], in0=xa[:, po + j:po + j + n],
                                                                           scalar=cw[:, c, j:j + 1], in1=uu[:, s0:s0 + n],
                                                                           op0=ALU.mult, op1=ALU.add), r=[xa, cw, uu], w=[uu])
                            K.V(lambda: nc.vector.scalar_tensor_tensor(out=ub[:, s0:s0 + n], in0=xa[:, po + 3:po + 3 + n],
                                                                       scalar=cw[:, c, 3:4], in1=uu[:, s0:s0 + n],
                                                                       op0=ALU.mult, op1=ALU.add), r=[xa, cw, uu], w=[ub])

                    def mainp(c):
                        wx_, wy_, bdA, bdX, ub = wxs[c % 2], wys[c % 2], bdAs[c % 2], bdXs[c % 2], ubs[c % 2]
                        segs = [(0, 256), (256, 2048), (2304, 2048)]
                        for d in range(2):
                            order = segs if d == 0 else [segs[0], segs[2], segs[1]]
                            for si, (s0, n) in enumerate(order):
                                rr, ii, a2 = rr_l[sgi_[0] % 2], ii_l[sgi_[0] % 2], a2_l[sgi_[0] % 2]
                                sgi_[0] += 1
                                for off in range(0, n, 512):
                                    bs = min(512, n - off)
                                    b1 = K.bank()
                                    K.P(lambda: nc.tensor.matmul(pb[:, b1, :bs], lhsT=bdA[:, d, :], rhs=ub[:, s0 + off:s0 + off + bs],
                                                                 start=True, stop=True), r=[bdA, ub], w=[pres[b1]])
                                    K.A(lambda: nc.scalar.activation(out=rr[:, off:off + bs], in_=pb[:, b1, :bs], func=AF.Sigmoid,
                                                                     bias=ba[:, d, c:c + 1]), r=[pres[b1], ba], w=[rr])
                                    b2 = K.bank()
                                    K.P(lambda: nc.tensor.matmul(pb[:, b2, :bs], lhsT=bdX[:, d, :], rhs=ub[:, s0 + off:s0 + off + bs],
                                                                 start=True, stop=True), r=[bdX, ub], w=[pres[b2]])
                                    K.A(lambda: nc.scalar.activation(out=ii[:, off:off + bs], in_=pb[:, b2, :bs], func=AF.Sigmoid,
                                                                     bias=bx[:, d, c:c + 1]), r=[pres[b2], bx], w=[ii])
                                K.A(lambda: nc.scalar.activation(out=a2[:, :n], in_=rr[:, :n], func=AF.Exp, scale=sp2[:, d, c:c + 1]),
                                    r=[rr, sp2], w=[a2])
                                K.A(lambda: nc.scalar.activation(out=rr[:, :n], in_=rr[:, :n], func=AF.Exp, scale=sp1[:, d, c:c + 1]),
                                    r=[rr, sp1], w=[rr])
                                K.A(lambda: nc.scalar.activation(out=a2[:, :n], in_=a2[:, :n], func=AF.Sqrt, bias=1.0, scale=-1.0),
                                    r=[a2], w=[a2])
                                K.V(lambda: nc.vector.tensor_tensor(out=ii[:, :n], in0=ii[:, :n], in1=ub[:, s0:s0 + n], op=ALU.mult),
                                    r=[ii, ub], w=[ii])
                                K.V(lambda: nc.vector.tensor_tensor(out=ii[:, :n], in0=ii[:, :n], in1=a2[:, :n], op=ALU.mult),
                                    r=[ii, a2], w=[ii])
                                if d == 0:
                                    init = 0.0 if si == 0 else hs[:, s0 - 1:s0]
                                    K.V(lambda: nc.vector.tensor_tensor_scan(out=hs[:, s0:s0 + n], data0=rr[:, :n], data1=ii[:, :n],
                                                                             initial=init, op0=ALU.mult, op1=ALU.add),
                                        r=[rr, ii, hs], w=[hs])
                                else:
                                    init = 0.0 if si == 0 else stt[:, 0:1]
                                    K.V(lambda: nc.vector.tensor_tensor_scan(out=a2[:, 0:n][:, ::-1], data0=rr[:, 0:n][:, ::-1],
                                                                             data1=ii[:, 0:n][:, ::-1], initial=init,
                                                                             op0=ALU.mult, op1=ALU.add),
                                        r=[rr, ii, stt], w=[a2])
                                    K.V(lambda: nc.vector.tensor_copy(out=stt[:], in_=a2[:, 0:1]), r=[a2], w=[stt])
                                    K.G(lambda: nc.gpsimd.tensor_tensor(out=hs[:, s0:s0 + n], in0=hs[:, s0:s0 + n], in1=a2[:, :n], op=ALU.add),
                                        r=[hs, a2], w=[hs])

                    def tailp(c):
                        wx_, wy_, bdA, bdX, ub = wxs[c % 2], wys[c % 2], bdAs[c % 2], bdXs[c % 2], ubs[c % 2]
                        for bi, (t0, bs) in enumerate(TB):
                            b = K.bank()
                            for k in range(8):
                                K.P(lambda: nc.tensor.matmul(pb[:, b, :bs], lhsT=wy_[:, k, :], rhs=uT[:, k, t0:t0 + bs],
                                                             start=(k == 0), stop=(k == 7)), r=[wy_, uT], w=[pres[b]], inc=(k == 7))
                            K.A(lambda: nc.scalar.activation(out=gy[:, :bs], in_=pb[:, b, :bs], func=AF.Gelu_apprx_tanh), r=[pres[b]], w=[gy])
                            yo = yos[yoi[0] % 2]
                            yoi[0] += 1
                            K.V(lambda: nc.vector.tensor_tensor(out=yo[:, :bs], in0=gy[:, :bs], in1=hs[:, t0:t0 + bs], op=ALU.mult),
                                r=[gy, hs], w=[yo])
                            K.dma(K.sp, yaT[c * 128:(c + 1) * 128, t0:t0 + bs], yo[:, :bs], r=[yo], dres=yo)


                    startup(0)
                    for c in range(8):
                        if c + 1 < 8:
                            startup(c + 1)
                        mainp(c)
                        tailp(c)

                chk("P2a")
                with K.phase():
                    wgs = [K.sb(f"wg{i}", [128, 8, 128], BF16) for i in range(2)]
                    obs = [K.sb(f"ob{i}", [128, T], BF16) for i in range(2)]
                    pb = K.ps("pb", [128, 8, 512], F32)
                    pres = [K.res() for _ in range(8)]
                    win = D["w_in"][l].rearrange("(k p) n -> p k n", p=128)
                    for cc in range(24):
                        wg, ob = wgs[cc % 2], obs[cc % 2]
                        K.dma(K.pool, wg[:], win[:, :, 6656 + cc * 128:6656 + (cc + 1) * 128], w=[wg], dres=wg)
                        for bi, (t0, bs) in enumerate(TB):
                            b = K.bank()
                            for k in range(8):
                                K.P(lambda: nc.tensor.matmul(pb[:, b, :bs], lhsT=wg[:, k, :], rhs=uT[:, k, t0:t0 + bs],
                                                             start=(k == 0), stop=(k == 7)), r=[wg, uT], w=[pres[b]], inc=(k == 7))
                            K.A(lambda: nc.scalar.activation(out=ob[:, t0:t0 + bs], in_=pb[:, b, :bs], func=AF.Sigmoid), r=[pres[b]], w=[ob])
                        K.dma(K.sp, sgT[cc * 128:(cc + 1) * 128, :], ob[:], r=[ob], dres=ob)

                chk("P2b")
                with K.phase():
                    Wt = K.sb("Wt", [128, 8, 4608], BF16)
                    win = D["w_in"][l].rearrange("(k p) n -> p k n", p=128)
                    for g in range(9):
                        K.dma(K.pool, Wt[:, :, g * 512:(g + 1) * 512], win[:, :, 2048 + g * 512:2048 + (g + 1) * 512], w=[Wt], dres=Wt)
                    ropes = [K.sb(f"rope{i}", [128, 256], F32) for i in range(2)]
                    xs_l = [K.sb(f"xs{i}", [128, 512], F32) for i in range(3)]
                    xsw_l = [K.sb(f"xsw{i}", [128, 512], F32) for i in range(3)]
                    t1_l = [K.sb(f"t1{i}", [128, 512], F32) for i in range(3)]
                    t2_l = [K.sb(f"t2{i}", [128, 512], F32) for i in range(3)]
                    rpi = [0]
                    NB = 2
                    o_q = [K.sb(f"oq{i}", [128, 512], BF16) for i in range(NB)]
                    o_k = [K.sb(f"ok{i}", [128, 512], BF16) for i in range(NB)]
                    o_v = [K.sb(f"ov{i}", [128, 1024], BF16) for i in range(NB)]
                    o_g = [K.sb(f"og{i}", [128, 1024], BF16) for i in range(NB)]
                    o_cq = [K.sb(f"ocq{i}", [128, 1024], BF16) for i in range(NB)]
                    o_ck = [K.sb(f"ock{i}", [128, 256], BF16) for i in range(NB)]
                    o_cv = [K.sb(f"ocv{i}", [128, 256], BF16) for i in range(NB)]
                    tq = [K.sb(f"tq{i}", [128, 4, 128], BF16) for i in range(NB)]
                    tk = [K.sb(f"tk{i}", [128, 4, 128], BF16) for i in range(NB)]
                    tcq = [K.sb(f"tcq{i}", [128, 8, 128], BF16) for i in range(NB)]
                    tck = [K.sb(f"tck{i}", [128, 2, 128], BF16) for i in range(NB)]
                    pb = K.ps("pb", [128, 6, 512], F32)
                    pres = [K.res() for _ in range(6)]
                    pst = K.ps("pst", [128, 2, 8, 128], BF16)
                    pstr = [K.res(), K.res()]
                    bki = [0]
                    psti = [0]

                    def nb():
                        bki[0] = (bki[0] + 1) % 6
                        return bki[0]

                    def proj(n, g):
                        b = nb()
                        for k in range(8):
                            K.P(lambda: nc.tensor.matmul(pb[:, b, :], lhsT=uT[:, k, n * 128:(n + 1) * 128], rhs=Wt[:, k, g * 512:(g + 1) * 512],
                                                         start=(k == 0), stop=(k == 7)), r=[uT, Wt], w=[pres[b]], inc=(k == 7))
                        pcount[0] += 1
                        flush()
                        return b

                    def rope(b, ncol, hsz, cos_ap, sin_ap, scale, out_t, ooff, ropet):
                        G = ncol // (2 * hsz)
                        H = ncol // 64
                        xs, xsw, t1, t2 = xs_l[rpi[0] % 3], xsw_l[rpi[0] % 3], t1_l[rpi[0] % 3], t2_l[rpi[0] % 3]
                        rpi[0] += 1
                        K.A(lambda: nc.scalar.activation(out=xs[:, :ncol], in_=pb[:, b, :ncol], func=AF.Copy, scale=scale), r=[pres[b]], w=[xs])
                        pv = pb[:, b, :ncol].rearrange("p (g two x) -> p g two x", two=2, x=hsz)
                        xv = xsw[:, :ncol].rearrange("p (g two x) -> p g two x", two=2, x=hsz)
                        K.A(lambda: nc.scalar.activation(out=xv[:, :, 0, :], in_=pv[:, :, 1, :], func=AF.Copy, scale=scale), r=[pres[b]], w=[xsw])
                        K.A(lambda: nc.scalar.activation(out=xv[:, :, 1, :], in_=pv[:, :, 0, :], func=AF.Copy, scale=scale), r=[pres[b]], w=[xsw])
                        x3 = xs[:, :ncol].rearrange("p (h x) -> p h x", x=64)
                        w3 = xsw[:, :ncol].rearrange("p (h x) -> p h x", x=64)
                        a3 = t1[:, :ncol].rearrange("p (h x) -> p h x", x=64)
                        b3 = t2[:, :ncol].rearrange("p (h x) -> p h x", x=64)
                        K.V(lambda: nc.vector.tensor_tensor(out=a3, in0=x3, in1=bc(cos_ap.unsqueeze(1), [128, H, 64]), op=ALU.mult),
                            r=[xs, ropet], w=[t1])
                        K.V(lambda: nc.vector.tensor_tensor(out=b3, in0=w3, in1=bc(sin_ap.unsqueeze(1), [128, H, 64]), op=ALU.mult),
                            r=[xsw, ropet], w=[t2])
                        K.V(lambda: nc.vector.tensor_tensor(out=out_t[:, ooff:ooff + ncol], in0=t1[:, :ncol], in1=t2[:, :ncol], op=ALU.add),
                            r=[t1, t2], w=[out_t])

                    deferred = []
                    pcount = [0]

                    def flush(all_=False):
                        while deferred and (all_ or deferred[0][0] <= pcount[0] - 2):
                            _, args = deferred.pop(0)
                            tr_store_now(*args)

                    def tr_store(*args):
                        deferred.append((pcount[0], args))

                    def tr_store_now(src_t, nblk, dst_t, dram_view):
                        s = psti[0] % 2
                        psti[0] += 1
                        transposes(lambda i: src_t[:, i * 128:(i + 1) * 128], nblk, pst[:, s], pstr[s], [src_t])
                        K.V(lambda: nc.vector.tensor_copy(out=dst_t[:, :nblk, :], in_=pst[:, s, :nblk, :]), r=[pstr[s]], w=[dst_t])
                        K.dma(K.sp, dram_view, dst_t[:, :nblk, :], r=[dst_t], dres=dst_t)

                    for n in range(NT):
                        t0 = n * 128
                        s = n % NB
                        rp = ropes[n % 2]
                        if n == 0:
                            K.dma(K.sp, rp[:], D["rope"][0], w=[rp], dres=rp)
                        if n + 1 < NT:
                            K.dma(K.sp, ropes[(n + 1) % 2][:], D["rope"][n + 1], w=[ropes[(n + 1) % 2]], dres=ropes[(n + 1) % 2])
                        cosR, sinR, cosA, sinA = rp[:, 0:64], rp[:, 64:128], rp[:, 128:192], rp[:, 192:256]
                        b = proj(n, 0)
                        rope(b, 512, 32, cosR, sinR, 1.0, o_q[s], 0, rp)
                        tr_store(o_q[s], 4, tq[s], rqT.rearrange("(c p) t -> p c t", p=128)[:, :, t0:t0 + 128])
                        b = proj(n, 1)
                        rope(b, 512, 32, cosR, sinR, 0.125, o_k[s], 0, rp)
                        K.dma(K.sp, rk[t0:t0 + 128, :], o_k[s][:], r=[o_k[s]], dres=o_k[s])
                        tr_store(o_k[s], 4, tk[s], rkT.rearrange("(c p) t -> p c t", p=128)[:, :, t0:t0 + 128])
                        for hh in range(2):
                            b = proj(n, 2 + hh)
                            K.A(lambda: nc.scalar.copy(out=o_v[s][:, hh * 512:(hh + 1) * 512], in_=pb[:, b, :]), r=[pres[b]], w=[o_v[s]])
                        K.dma(K.sp, rv[t0:t0 + 128, :], o_v[s][:], r=[o_v[s]], dres=o_v[s])
                        for hh in range(2):
                            b = proj(n, 4 + hh)
                            K.A(lambda: nc.scalar.activation(out=o_g[s][:, hh * 512:(hh + 1) * 512], in_=pb[:, b, :], func=AF.Silu),
                                r=[pres[b]], w=[o_g[s]])
                        K.dma(K.sp, rg[t0:t0 + 128, :], o_g[s][:], r=[o_g[s]], dres=o_g[s])
                        for hh in range(2):
                            b = proj(n, 6 + hh)
                            rope(b, 512, 16, cosA, sinA, 0.125, o_cq[s], hh * 512, rp)
                        tr_store(o_cq[s], 8, tcq[s], aqT.rearrange("(c p) t -> p c t", p=128)[:, :, t0:t0 + 128])
                        b = proj(n, 8)
                        rope(b, 256, 16, cosA, sinA, 1.0, o_ck[s], 0, rp)
                        K.A(lambda: nc.scalar.copy(out=o_cv[s][:], in_=pb[:, b, 256:512]), r=[pres[b]], w=[o_cv[s]])
                        K.dma(K.sp, av[t0:t0 + 128, :], o_cv[s][:], r=[o_cv[s]], dres=o_cv[s])
                        tr_store(o_ck[s], 2, tck[s], akT.rearrange("(c p) t -> p c t", p=128)[:, :, t0:t0 + 128])
                    flush(True)

            chk("P2c")
            with K.phase():
                kT = K.sb("kT", [64, 4, T], BF16)
                vaug = K.sb("vaug", [128, NT, 4, 65], BF16)
                esk = K.sb("esk", [128, 16], F32)
                K.dma(K.sp, kT[:], akT.rearrange("(g d) t -> d g t", d=64), w=[kT], dres=kT)
                for g in range(4):
                    K.dma(K.sp, vaug[:, :, g, 0:64], av.rearrange("(n p) (g d) -> p n g d", p=128, d=64)[:, :, g, :], w=[vaug], dres=vaug)
                K.G(lambda: nc.gpsimd.memset(vaug[:, :, :, 64:65], 1.0), w=[vaug])
                K.dma(K.sp, esk[:], D["sink_rep"][:, l], w=[esk], dres=esk)
                K.A(lambda: nc.scalar.activation(out=esk[:], in_=esk[:], func=AF.Exp), r=[esk], w=[esk])
                qbs = [K.sb(f"qb{i}", [64, 2048], BF16) for i in range(2)]
                pTs = [K.sb(f"pT{i}", [128, 5, 512], BF16) for i in range(3)]
                ycs = [K.sb(f"yc{i}", [128, 1024], BF16) for i in range(2)]
                ycts = [K.sb(f"yct{i}", [128, 8, 128], BF16) for i in range(2)]
                dens = [K.sb(f"den{i}", [128, 4], F32) for i in range(2)]
                recs = [K.sb(f"rec{i}", [128, 4], F32) for i in range(2)]
                pb = K.ps("pb", [128, 4, 512], F32)
                pres = [K.res() for _ in range(4)]
                po = K.ps("po", [128, 2, 4, 128], F32)
                por = [K.res(), K.res()]
                pst = K.ps("pst", [128, 8, 128], BF16)
                pstr = K.res()
                aq3 = aqT.rearrange("(h d) t -> d h t", d=64)
                sbi = [0]

                def keys_of(n):
                    if n < 2:
                        return [(0, None), (1, None)]
                    keys = []
                    if n - 1 >= 2:
                        keys.append((n - 1, maskp))
                    keys.append((n, None))
                    if n + 1 < NT:
                        keys.append((n + 1, maskn))
                    return keys + [(0, None), (1, None)]

                def load_q(n):
                    qb = qbs[n % 2]
                    K.dma(K.sp, qb[:].rearrange("d (h t) -> d h t", t=128), aq3[:, :, n * 128:(n + 1) * 128], w=[qb], dres=qb)

                def emit_scores(n, g, gi):
                    qb = qbs[n % 2]
                    pT = pTs[gi % 3]
                    for si, (kt, m) in enumerate(keys_of(n)):
                        bk = sbi[0] % 4
                        sbi[0] += 1
                        K.P(lambda: nc.tensor.matmul(pb[:, bk, :], lhsT=kT[:, g, kt * 128:(kt + 1) * 128], rhs=qb[:, g * 512:(g + 1) * 512],
                                                     start=True, stop=True), r=[kT, qb], w=[pres[bk]])
                        K.A(lambda: nc.scalar.activation(out=pT[:, si, :], in_=pb[:, bk, :], func=AF.Exp), r=[pres[bk]], w=[pT])
                        if m is not None:
                            pv = pT[:, si, :].rearrange("p (r i) -> p r i", i=128)
                            K.G(lambda: nc.gpsimd.tensor_tensor(out=pv, in0=pv, in1=bc(m[:, :].unsqueeze(1), [128, 4, 128]), op=ALU.mult),
                                r=[pT, m], w=[pT])

                def emit_pv(n, g, gi):
                    pT = pTs[gi % 3]
                    pos = gi % 2
                    yc = ycs[n % 2]
                    den, rec = dens[gi % 2], recs[gi % 2]
                    keys = keys_of(n)
                    for r_ in range(4):
                        for si, (kt, m) in enumerate(keys):
                            K.P(lambda: nc.tensor.matmul(po[:, pos, r_, 0:65], lhsT=pT[:, si, r_ * 128:(r_ + 1) * 128], rhs=vaug[:, kt, g, :],
                                                         start=(si == 0), stop=(si == len(keys) - 1)),
                                r=[pT, vaug], w=[por[pos]], inc=(si == len(keys) - 1))
                    K.V(lambda: nc.vector.tensor_tensor(out=den[:], in0=po[:, pos, :, 64], in1=esk[:, g * 4:(g + 1) * 4], op=ALU.add),
                        r=[por[pos], esk], w=[den])
                    K.V(lambda: nc.vector.reciprocal(out=rec[:], in_=den[:]), r=[den], w=[rec])
                    K.V(lambda: nc.vector.tensor_tensor(out=yc[:, g * 256:(g + 1) * 256].rearrange("p (r d) -> p r d", d=64),
                                                        in0=po[:, pos, :, 0:64], in1=bc(rec[:, :].unsqueeze(2), [128, 4, 64]), op=ALU.mult),
                        r=[por[pos], rec], w=[yc])
                    if g == 3:
                        t0 = n * 128
                        transposes(lambda i: yc[:, i * 128:(i + 1) * 128], 8, pst, pstr, [yc])
                        yct = ycts[n % 2]
                        K.V(lambda: nc.vector.tensor_copy(out=yct[:], in_=pst[:]), r=[pstr], w=[yct])
                        K.dma(K.sp, ycT.rearrange("(c p) t -> p c t", p=128)[:, :, t0:t0 + 128], yct[:], r=[yct], dres=yct)

                do_mod = (l + 1 < n_layers)
                if do_mod:
                    wm3 = [K.sb(f"wm3_{i}", [128, 8, 128], F32) for i in range(6)]
                    pm3 = K.ps("pm3", [128, 48, 2], F32)
                load_q(0)
                pending = None
                gi = 0
                for n in range(NT):
                    if n + 1 < NT:
                        load_q(n + 1)
                    if do_mod and n == 26:
                        mod_finish(l + 1, pm3)
                    for g in range(4):
                        if do_mod and gi >= 8 and gi % 2 == 0 and (gi - 8) // 2 < 48:
                            mj = (gi - 8) // 2
                            mod_chunk_small(l + 1, mj, wm3[mj % 6], pm3)
                        emit_scores(n, g, gi)
                        if pending is not None:
                            emit_pv(*pending)
                        pending = (n, g, gi)
                        gi += 1
                emit_pv(*pending)

            chk("P3")
            with K.phase():
                lr = K.sb("lr", [128, 2, 8], F32)
                lS = K.sb("lS", [128, 2, 4], F32)
                K.dma(K.sp, lr[:], D["ret_lam_rep"][:, l], w=[lr], dres=lr)
                K.dma(K.sp, lS[:], D["ret_lam_S"][:, l], w=[lS], dres=lS)
                for tt in (lr, lS):
                    K.A(lambda: nc.scalar.activation(out=tt[:], in_=tt[:], func=AF.Exp, scale=-1.0), r=[tt], w=[tt])
                    K.A(lambda: nc.scalar.activation(out=tt[:], in_=tt[:], func=AF.Ln, bias=1.0), r=[tt], w=[tt])
                    K.V(lambda: nc.vector.tensor_scalar(out=tt[:], in0=tt[:], scalar1=-1.0, scalar2=None, op0=ALU.mult), r=[tt], w=[tt])
                G128 = K.sb("G128", [128, 2, 4], F32)
                K.A(lambda: nc.scalar.activation(out=G128[:], in_=lS[:], func=AF.Exp, scale=128.0), r=[lS], w=[G128])
                dec = K.sb("dec", [128, 4, 8], F32)
                cv = lambda j: cst[:, C_CV + j:C_CV + j + 1]
                K.A(lambda: nc.scalar.activation(out=dec[:, 0, :], in_=lr[:, 0, :], func=AF.Exp, scale=cv(1)), r=[lr, cst], w=[dec])
                K.A(lambda: nc.scalar.activation(out=dec[:, 1, :], in_=lr[:, 1, :], func=AF.Exp, scale=cv(0)), r=[lr, cst], w=[dec])
                K.A(lambda: nc.scalar.activation(out=dec[:, 2, :], in_=lr[:, 0, :], func=AF.Exp, scale=cv(2)), r=[lr, cst], w=[dec])
                K.A(lambda: nc.scalar.activation(out=dec[:, 3, :], in_=lr[:, 1, :], func=AF.Exp, scale=cv(3)), r=[lr, cst], w=[dec])
                DT = K.sb("DT", [128, 8, 128], F32)
                e1 = K.sb("e1", [128, 128], F32)
                e2 = K.sb("e2", [128, 128], F32)
                for h in range(8):
                    K.A(lambda: nc.scalar.activation(out=e1[:], in_=cst[:, C_R1:C_R1 + 128], func=AF.Exp, scale=lr[:, 0, h:h + 1]), r=[cst, lr], w=[e1])
                    K.A(lambda: nc.scalar.activation(out=e2[:], in_=cst[:, C_R2:C_R2 + 128], func=AF.Exp, scale=lr[:, 1, h:h + 1]), r=[cst, lr], w=[e2])
                    K.V(lambda: nc.vector.tensor_tensor(out=e1[:], in0=e1[:], in1=cst[:, C_M1:C_M1 + 128], op=ALU.mult), r=[e1, cst], w=[e1])
                    K.V(lambda: nc.vector.tensor_tensor(out=e2[:], in0=e2[:], in1=cst[:, C_M2:C_M2 + 128], op=ALU.mult), r=[e2, cst], w=[e2])
                    K.V(lambda: nc.vector.tensor_tensor(out=e1[:], in0=e1[:], in1=e2[:], op=ALU.add), r=[e1, e2], w=[e1])
                    K.V(lambda: nc.vector.tensor_tensor(out=DT[:, h, :], in0=e1[:], in1=cst[:, C_I2:C_I2 + 128], op=ALU.add), r=[e1, cst], w=[DT])
                Sst = K.sb("Sst", [128, 4, 128], F32)
                Sbf = K.sb("Sbf", [128, 4, 128], BF16)
                Stm = K.sb("Stm", [128, 4, 128], F32)
                kts = [K.sb(f"kt{i}", [128, 512], BF16) for i in range(2)]
                vts = [K.sb(f"vt{i}", [128, 1024], BF16) for i in range(2)]
                kd = K.sb("kd", [128, 512], BF16)
                pb = K.ps("pb", [128, 7, 512], F32)
                pres = [K.res() for _ in range(7)]
                pst4 = K.ps("pst4", [128, 8, 128], BF16)
                pst4r = K.res()

                def state_update(d, kt_, vt_, bank):
                    K.G(lambda: nc.gpsimd.tensor_tensor(out=kd[:].rearrange("p (h x) -> p h x", x=64),
                                                        in0=kt_[:].rearrange("p (h x) -> p h x", x=64),
                                                        in1=bc(dec[:, d, :].unsqueeze(2), [128, 8, 64]), op=ALU.mult),
                        r=[kt_, dec], w=[kd])
                    for h in range(8):
                        K.P(lambda: nc.tensor.matmul(pb[(h % 2) * 64:(h % 2) * 64 + 64, bank, (h // 2) * 128:(h // 2) * 128 + 128],
                                                     lhsT=kd[:, h * 64:(h + 1) * 64], rhs=vt_[:, h * 128:(h + 1) * 128], start=True, stop=True),
                            r=[kd, vt_], w=[pres[bank]], inc=(h == 7))
                    K.V(lambda: nc.vector.tensor_tensor(out=Stm[:], in0=Sst[:], in1=bc(G128[:, d, :].unsqueeze(2), [128, 4, 128]), op=ALU.mult),
                        r=[Sst, G128], w=[Stm])
                    K.V(lambda: nc.vector.tensor_tensor(out=Sst[:], in0=Stm[:], in1=pb[:, bank, :].rearrange("p (a e) -> p a e", e=128), op=ALU.add),
                        r=[Stm, pres[bank]], w=[Sst])
                    K.G(lambda: nc.gpsimd.tensor_copy(out=Sbf[:], in_=Sst[:]), r=[Sst], w=[Sbf])

                K.V(lambda: nc.vector.memset(Sst[:], 0.0), w=[Sst])
                K.V(lambda: nc.vector.memset(Sbf[:], 0.0), w=[Sbf])
                def loads1(n_):
                    K.dma(K.sp, kts[n_ % 2][:], rk[n_ * 128:(n_ + 1) * 128, :], w=[kts[n_ % 2]], dres=kts[n_ % 2])
                    K.dma(K.sp, vts[n_ % 2][:], rv[n_ * 128:(n_ + 1) * 128, :], w=[vts[n_ % 2]], dres=vts[n_ % 2])
                loads1(0)
                for n in range(NT):
                    t0 = n * 128
                    kt_, vt_ = kts[n % 2], vts[n % 2]
                    if n + 1 < NT:
                        loads1(n + 1)
                    K.dma(K.sp, SfD[n].rearrange("p (a e) -> p a e", e=128), Sbf[:], r=[Sbf], dres=Sbf)
                    state_update(0, kt_, vt_, n % 2)
                K.barrier()
                qTs = [K.sb(f"qT{i}", [128, 4, 128], BF16) for i in range(2)]
                kTs = [K.sb(f"kTt{i}", [128, 4, 128], BF16) for i in range(2)]
                gts = [K.sb(f"gt{i}", [128, 1024], BF16) for i in range(3)]
                Sfs = [K.sb(f"Sf{i}", [128, 4, 128], BF16) for i in range(2)]
                AT_l = [K.sb(f"AT{i}", [128, 8, 128], BF16) for i in range(2)]
                ysb_l = [K.sb(f"ysb{i}", [128, 1024], F32) for i in range(2)]
                ytm_l = [K.sb(f"ytm{i}", [128, 1024], F32) for i in range(2)]
                ysq_l = [K.sb(f"ysq{i}", [128, 1024], F32) for i in range(2)]
                s1_l = [K.sb(f"s1{i}", [128, 8], F32) for i in range(2)]
                s2_l = [K.sb(f"s2{i}", [128, 8], F32) for i in range(2)]
                mu_l = [K.sb(f"mu{i}", [128, 8], F32) for i in range(2)]
                var_l = [K.sb(f"var{i}", [128, 8], F32) for i in range(2)]
                ybs = [K.sb(f"yb{i}", [128, 1024], BF16) for i in range(2)]
                ybt = [K.sb(f"ybt{i}", [128, 8, 128], BF16) for i in range(2)]
                K.V(lambda: nc.vector.memset(Sst[:], 0.0), w=[Sst])
                K.V(lambda: nc.vector.memset(Sbf[:], 0.0), w=[Sbf])
                order = [1, 0] + list(range(NT - 1, 1, -1))

                def loads2(it_):
                    n_ = order[it_]
                    s_ = it_ % 2
                    t0_ = n_ * 128
                    K.dma(K.sp, qTs[s_][:], rqT.rearrange("(c p) t -> p c t", p=128)[:, :, t0_:t0_ + 128], w=[qTs[s_]], dres=qTs[s_])
                    K.dma(K.sp, kTs[s_][:], rkT.rearrange("(c p) t -> p c t", p=128)[:, :, t0_:t0_ + 128], w=[kTs[s_]], dres=kTs[s_])
                    K.dma(K.sp, kts[s_][:], rk[t0_:t0_ + 128, :], w=[kts[s_]], dres=kts[s_])
                    K.dma(K.sp, vts[s_][:], rv[t0_:t0_ + 128, :], w=[vts[s_]], dres=vts[s_])
                    K.dma(K.sp, Sfs[s_][:], SfD[n_].rearrange("p (a e) -> p a e", e=128), w=[Sfs[s_]], dres=Sfs[s_])
                    K.dma(K.sp, gts[it_ % 3][:], rg[t0_:t0_ + 128, :], w=[gts[it_ % 3]], dres=gts[it_ % 3])
                def stageA(it):
                    n = order[it]
                    s = it % 2
                    kt_, vt_, qT_, kT_, Sf_ = kts[s], vts[s], qTs[s], kTs[s], Sfs[s]
                    AT, ysb = AT_l[s], ysb_l[s]
                    for h in range(8):
                        po_ = (h % 2) * 64
                        sl = slice((h // 2) * 128, (h // 2) * 128 + 128)
                        K.P(lambda: nc.tensor.matmul(pb[:, h % 2, sl], lhsT=kT_[po_:po_ + 64, h // 2, :],
                                                     rhs=qT_[po_:po_ + 64, h // 2, :], start=True, stop=True),
                            r=[kT_, qT_], w=[pres[h % 2]])
                    for par in range(2):
                        K.V(lambda: nc.vector.tensor_tensor(out=AT[:, par::2, :],
                                                            in0=pb[:, par, :].rearrange("p (a e) -> p a e", e=128),
                                                            in1=DT[:, par::2, :], op=ALU.mult),
                            r=[pres[par], DT], w=[AT])
                    for h in range(8):
                        sl = slice((h // 2) * 128, (h // 2) * 128 + 128)
                        K.P(lambda: nc.tensor.matmul(pb[:, 2 + h % 2, sl], lhsT=AT[:, h, :], rhs=vt_[:, h * 128:(h + 1) * 128], start=True, stop=True),
                            r=[AT, vt_], w=[pres[2 + h % 2]])
                    for h in range(8):
                        po_ = (h % 2) * 64
                        sl = slice((h // 2) * 128, (h // 2) * 128 + 128)
                        K.P(lambda: nc.tensor.matmul(pb[:, 4 + h % 2, sl], lhsT=qT_[po_:po_ + 64, h // 2, :], rhs=Sf_[po_:po_ + 64, h // 2, :],
                                                     start=True, stop=True), r=[qT_, Sf_], w=[pres[4 + h % 2]])
                    for h in range(8):
                        po_ = (h % 2) * 64
                        sl = slice((h // 2) * 128, (h // 2) * 128 + 128)
                        K.P(lambda: nc.tensor.matmul(pb[:, h % 2, sl], lhsT=qT_[po_:po_ + 64, h // 2, :], rhs=Sbf[po_:po_ + 64, h // 2, :],
                                                     start=True, stop=True), r=[qT_, Sbf], w=[pres[h % 2]])
                    for par in range(2):
                        hs_ = slice(par * 512, (par + 1) * 512)
                        K.A(lambda: nc.scalar.copy(out=ysb[:, hs_], in_=pb[:, 2 + par, :]), r=[pres[2 + par]], w=[ysb])
                    for h in range(8):
                        par, pr = h % 2, h // 2
                        cs_ = slice(par * 512 + pr * 128, par * 512 + pr * 128 + 128)
                        ps_ = slice(pr * 128, pr * 128 + 128)
                        K.V(lambda: nc.vector.scalar_tensor_tensor(out=ysb[:, cs_], in0=pb[:, 4 + par, ps_], scalar=dec[:, 2, h:h + 1],
                                                                   in1=ysb[:, cs_], op0=ALU.mult, op1=ALU.add),
                            r=[pres[4 + par], dec, ysb], w=[ysb])
                        K.V(lambda: nc.vector.scalar_tensor_tensor(out=ysb[:, cs_], in0=pb[:, par, ps_], scalar=dec[:, 3, h:h + 1],
                                                                   in1=ysb[:, cs_], op0=ALU.mult, op1=ALU.add),
                            r=[pres[par], dec, ysb], w=[ysb])
                    state_update(1, kt_, vt_, 6)
                def stageB(it):
                    n = order[it]
                    t0 = n * 128
                    s = it % 2
                    gt_ = gts[it % 3]
                    ysb, ysq, s1, s2, mu, var = ysb_l[s], ysq_l[s], s1_l[s], s2_l[s], mu_l[s], var_l[s]
                    y3 = ysb[:].rearrange("p (h e) -> p h e", e=128)
                    for hp in range(8):
                        hsl = slice(hp * 128, (hp + 1) * 128)
                        K.A(lambda: nc.scalar.activation(out=ysq[:, hsl], in_=ysb[:, hsl], func=AF.Identity, accum_out=s1[:, hp:hp + 1]),
                            r=[ysb], w=[ysq, s1])
                        K.A(lambda: nc.scalar.activation(out=ysq[:, hsl], in_=ysb[:, hsl], func=AF.Square, accum_out=s2[:, hp:hp + 1]),
                            r=[ysb], w=[ysq, s2])
                    K.V(lambda: nc.vector.tensor_scalar(out=mu[:], in0=s1[:], scalar1=1.0 / 128.0, scalar2=None, op0=ALU.mult), r=[s1], w=[mu])
                    K.V(lambda: nc.vector.tensor_tensor(out=var[:], in0=mu[:], in1=mu[:], op=ALU.mult), r=[mu], w=[var])
                    K.V(lambda: nc.vector.scalar_tensor_tensor(out=var[:], in0=s2[:], scalar=1.0 / 128.0, in1=var[:], op0=ALU.mult, op1=ALU.subtract),
                        r=[s2, var], w=[var])
                    K.A(lambda: nc.scalar.activation(out=var[:], in_=var[:], func=AF.Sqrt, bias=epsT[:, 0:1]), r=[var, epsT], w=[var])
                    K.V(lambda: nc.vector.reciprocal(out=var[:], in_=var[:]), r=[var], w=[var])
                    K.G(lambda: nc.gpsimd.tensor_tensor(out=y3, in0=y3, in1=bc(mu[:, :].unsqueeze(2), [128, 8, 128]), op=ALU.subtract),
                        r=[ysb, mu], w=[ysb])
                    yb_ = ybs[s]
                    for h in range(8):
                        hp = (h % 2) * 4 + h // 2
                        K.V(lambda: nc.vector.scalar_tensor_tensor(out=yb_[:, h * 128:(h + 1) * 128], in0=ysb[:, hp * 128:(hp + 1) * 128],
                                                                   scalar=var[:, hp:hp + 1], in1=gt_[:, h * 128:(h + 1) * 128],
                                                                   op0=ALU.mult, op1=ALU.mult),
                            r=[ysb, var, gt_], w=[yb_])
                    for i in range(8):
                        K.P(lambda: nc.tensor.transpose(out=pst4[:, i, :], in_=yb_[:, i * 128:(i + 1) * 128], identity=identb[:]),
                            r=[yb_, identb], w=[pst4r], inc=(i == 7))
                    K.A(lambda: nc.scalar.copy(out=ybt[s][:], in_=pst4[:]), r=[pst4r], w=[ybt[s]])
                    K.dma(K.sp, ybT.rearrange("(c p) t -> p c t", p=128)[:, :, t0:t0 + 128], ybt[s][:], r=[ybt[s]], dres=ybt[s])

                loads2(0)
                for it in range(len(order)):
                    if it + 1 < len(order):
                        loads2(it + 1)
                    stageA(it)
                    if it >= 1:
                        stageB(it - 1)
                stageB(len(order) - 1)

            chk("P4")

            with K.phase():
                Ws = []
                for nm in ("w_ba", "w_bb", "w_bc", "w_out"):
                    wt = K.sb(nm, [128, 8, 1024], BF16)
                    for hh in range(2):
                        K.dma(K.pool, wt[:, :, hh * 512:(hh + 1) * 512],
                              D[nm][l].rearrange("(k p) n -> p k n", p=128)[:, :, hh * 512:(hh + 1) * 512], w=[wt], dres=wt)
                    Ws.append(wt)
                Wa, Wb, Wc, Wo = Ws
                yas = [K.sb(f"ya{i}", [128, 8, 512], BF16) for i in range(2)]
                ybs_ = [K.sb(f"ybm{i}", [128, 8, 512], BF16) for i in range(2)]
                ycs_ = [K.sb(f"ycm{i}", [128, 8, 512], BF16) for i in range(2)]
                sgs = [K.sb(f"sg{i}", [128, 24, 512], BF16) for i in range(1)] * 2
                hbs = [K.sb(f"hb{i}", [128, 8, 512], F32) for i in range(2)]
                mT = K.sb("mT", [128, 8, 512], BF16)
                m1 = K.sb("m1", [128, 512], F32)
                m2 = K.sb("m2", [128, 512], F32)
                m3 = K.sb("m3", [128, 512], F32)
                pb = K.ps("pb", [128, 8, 512], F32)
                pres = [K.res() for _ in range(8)]
                def loads5(bi_):
                    t0_, bs_ = TB[bi_]
                    s_ = bi_ % 2
                    K.dma(K.sp, yas[s_][:, :, :bs_], fm(yaT, t0_, bs_), w=[yas[s_]], dres=yas[s_])
                    K.dma(K.sp, ybs_[s_][:, :, :bs_], fm(ybT, t0_, bs_), w=[ybs_[s_]], dres=ybs_[s_])
                    K.dma(K.sp, ycs_[s_][:, :, :bs_], fm(ycT, t0_, bs_), w=[ycs_[s_]], dres=ycs_[s_])
                    K.dma(K.sp, hbs[s_][:, :, :bs_], fm(hT, t0_, bs_), w=[hbs[s_]], dres=hbs[s_])

                def loadsg(bi_):
                    t0_, bs_ = TB[bi_]
                    K.dma(K.sp, sgs[0][:, :, :bs_], sgT.rearrange("(k p) t -> p k t", p=128)[:, :, t0_:t0_ + bs_], w=[sgs[0]], dres=sgs[0])
                loads5(0)
                for bi, (t0, bs) in enumerate(TB):
                    s = bi % 2
                    v = 1 if bi == 0 else 0
                    loadsg(bi)
                    if bi + 1 < len(TB):
                        loads5(bi + 1)
                    for nn in range(8):
                        bks = []
                        for (W_, y_) in ((Wa, yas[s]), (Wb, ybs_[s]), (Wc, ycs_[s])):
                            b = K.bank()
                            bks.append(b)
                            for k in range(8):
                                K.P(lambda: nc.tensor.matmul(pb[:, b, :bs], lhsT=W_[:, k, nn * 128:(nn + 1) * 128], rhs=y_[:, k, :bs],
                                                             start=(k == 0), stop=(k == 7)), r=[W_, y_], w=[pres[b]], inc=(k == 7))
                        K.V(lambda: nc.vector.tensor_tensor(out=m1[:, :bs], in0=pb[:, bks[0], :bs], in1=sgs[s][:, nn, :bs], op=ALU.mult),
                            r=[pres[bks[0]], sgs[s]], w=[m1])
                        K.V(lambda: nc.vector.tensor_tensor(out=m2[:, :bs], in0=pb[:, bks[1], :bs], in1=sgs[s][:, 8 + nn, :bs], op=ALU.mult),
                            r=[pres[bks[1]], sgs[s]], w=[m2])
                        K.V(lambda: nc.vector.tensor_tensor(out=m3[:, :bs], in0=pb[:, bks[2], :bs], in1=sgs[s][:, 16 + nn, :bs], op=ALU.mult),
                            r=[pres[bks[2]], sgs[s]], w=[m3])
                        K.G(lambda: nc.gpsimd.tensor_tensor(out=m1[:, :bs], in0=m1[:, :bs], in1=m2[:, :bs], op=ALU.add), r=[m1, m2], w=[m1])
                        K.G(lambda: nc.gpsimd.tensor_tensor(out=mT[:, nn, :bs], in0=m1[:, :bs], in1=m3[:, :bs], op=ALU.add), r=[m1, m3], w=[mT])
                    for nn in range(8):
                        b = K.bank()
                        for k in range(8):
                            K.P(lambda: nc.tensor.matmul(pb[:, b, :bs], lhsT=Wo[:, k, nn * 128:(nn + 1) * 128], rhs=mT[:, k, :bs],
                                                         start=(k == 0), stop=(k == 7)), r=[Wo, mT], w=[pres[b]], inc=(k == 7))
                        K.V(lambda: nc.vector.scalar_tensor_tensor(out=hbs[s][:, nn, :bs], in0=pb[:, b, :bs], scalar=mods(l, 2, nn, v),
                                                                   in1=hbs[s][:, nn, :bs], op0=ALU.mult, op1=ALU.add),
                            r=[pres[b], modT, hbs[s]], w=[hbs[s]])
                    K.dma(K.sp, fm(hT, t0, bs), hbs[s][:, :, :bs], r=[hbs[s]], dres=hbs[s])

            chk("P5")

            with K.phase():
                hbs = [K.sb(f"hb{i}", [128, 8, 512], F32) for i in range(2)]
                sqb = K.sb("sqb", [128, 8, 512], BF16)
                tmp = K.sb("tmp", [128, 8, 512], F32)
                v32 = K.sb("v32", [128, 8, 512], F32)
                rt = K.sb("rt", [128, 512], F32)
                rstd = K.sb("rstd", [128, 512], F32)
                wr = K.sb("wr", [128, 8, 36], F32)
                br = K.sb("br", [128, 36], F32)
                K.dma(K.sp, wr[:], D["w_rt"][:, l], w=[wr], dres=wr)
                K.dma(K.sp, br[:], D["b_rt"][:, l], w=[br], dres=br)
                run = K.sb("run", [128, 32], F32)
                K.V(lambda: nc.vector.memset(run[:], 0.0), w=[run])
                lg = K.sb("lg", [128, 36], F32)
                gmx = K.sb("gmx", [128, 1], F32)
                ngm = K.sb("ngm", [128, 1], F32)
                gex = K.sb("gex", [128, 4], F32)
                gsum = K.sb("gsum", [128, 1], F32)
                gw = K.sb("gw", [128, 1], F32)
                gmk = K.sb("gmk", [128, 4], F32)
                lem = K.sb("lem", [128, 32], F32)
                m8 = K.sb("m8", [128, 8], F32)
                mk1 = K.sb("mk1", [128, 32], F32)
                mk2 = K.sb("mk2", [128, 32], F32)
                mk12 = K.sb("mk12", [128, 32], BF16)
                dd = K.sb("dd", [128, 1], F32)
                posv = K.sb("posv", [128, 32], F32)
                sf = K.sb("sf", [128, 2], F32)
                junk = K.sb("junk", [128, 32], F32)
                rows = [K.sb(f"rows{i}", [128, 1024], BF16) for i in range(2)]
                pb = K.ps("pb", [128, 2, 512], F32)
                pres = [K.res(), K.res()]
                pr = K.ps("pr", [128, 2, 64], F32)
                prr = [K.res(), K.res()]
                pc = K.ps("pc", [128, 2, 32], F32)
                pcr = K.res()
                ptr = K.ps("ptr", [128, 2, 1024], F32)
                ptrr = [K.res(), K.res()]
                ti = 0
                for bi, (t0, bs) in enumerate(TB):
                    hb = hbs[bi % 2]
                    v = 1 if bi == 0 else 0
                    K.dma(K.sp, hb[:, :, :bs], fm(hT, t0, bs), w=[hb], dres=hb)
                    norm_block(hb, bs, sqb, tmp, rt, rstd, pb[:, bi % 2, :], pres[bi % 2])
                    for k in range(8):
                        K.A(lambda: nc.scalar.activation(out=v32[:, k, :bs], in_=tmp[:, k, :bs], func=AF.Identity,
                                                         bias=mods(l, 3, k, v), scale=gs2[:, k, v:v + 1]), r=[tmp, gs2, modT], w=[v32])
                    for tt in range(bs // 128):
                        n = t0 // 128 + tt
                        q = ti % 2
                        ti += 1
                        ts_ = slice(tt * 128, (tt + 1) * 128)
                        for k in range(8):
                            K.P(lambda: nc.tensor.matmul(pr[:, q, 0:36], lhsT=v32[:, k, ts_], rhs=wr[:, k, :], start=(k == 0), stop=(k == 7)),
                                r=[v32, wr], w=[prr[q]], inc=(k == 7))
                        for k in range(8):
                            K.P(lambda: nc.tensor.transpose(out=ptr[:, q, k * 128:(k + 1) * 128], in_=v32[:, k, ts_], identity=cst[:, C_ID:C_ID + 128]),
                                r=[v32, cst], w=[ptrr[q]], inc=(k == 7))
                        rw = rows[q]
                        K.A(lambda: nc.scalar.copy(out=rw[:], in_=ptr[:, q, :]), r=[ptrr[q]], w=[rw])
                        K.V(lambda: nc.vector.tensor_tensor(out=lg[:], in0=pr[:, q, 0:36], in1=br[:], op=ALU.add), r=[prr[q], br], w=[lg])
                        K.V(lambda: nc.vector.reduce_max(out=gmx[:], in_=lg[:, 0:4], axis=AX.X), r=[lg], w=[gmx])
                        K.V(lambda: nc.vector.tensor_scalar(out=ngm[:], in0=gmx[:], scalar1=-1.0, scalar2=None, op0=ALU.mult), r=[gmx], w=[ngm])
                        K.A(lambda: nc.scalar.activation(out=gex[:], in_=lg[:, 0:4], func=AF.Exp, bias=ngm[:, 0:1]), r=[lg, ngm], w=[gex])
                        K.V(lambda: nc.vector.reduce_sum(out=gsum[:], in_=gex[:], axis=AX.X), r=[gex], w=[gsum])
                        K.V(lambda: nc.vector.reciprocal(out=gw[:], in_=gsum[:]), r=[gsum], w=[gw])
                        K.V(lambda: nc.vector.tensor_scalar(out=gmk[:], in0=lg[:, 0:4], scalar1=gmx[:, 0:1], scalar2=None, op0=ALU.is_equal),
                            r=[lg, gmx], w=[gmk])
                        K.V(lambda: nc.vector.tensor_scalar(out=gex[:], in0=gmk[:], scalar1=1e30, scalar2=-1e30, op0=ALU.mult, op1=ALU.add),
                            r=[gmk], w=[gex])
                        K.V(lambda: nc.vector.tensor_tensor(out=lem[:].rearrange("p (g e) -> p g e", e=8),
                                                            in0=lg[:, 4:36].rearrange("p (g e) -> p g e", e=8),
                                                            in1=bc(gex[:, :].unsqueeze(2), [128, 4, 8]), op=ALU.add), r=[lg, gex], w=[lem])
                        K.V(lambda: nc.vector.max(out=m8[:], in_=lem[:]), r=[lem], w=[m8])
                        K.V(lambda: nc.vector.tensor_scalar(out=mk1[:], in0=lem[:], scalar1=m8[:, 0:1], scalar2=None, op0=ALU.is_equal),
                            r=[lem, m8], w=[mk1])
                        K.V(lambda: nc.vector.tensor_scalar(out=mk2[:], in0=lem[:], scalar1=m8[:, 1:2], scalar2=None, op0=ALU.is_equal),
                            r=[lem, m8], w=[mk2])
                        K.V(lambda: nc.vector.tensor_tensor(out=mk12[:], in0=mk1[:], in1=mk2[:], op=ALU.add), r=[mk1, mk2], w=[mk12])
                        K.V(lambda: nc.vector.tensor_tensor(out=dd[:], in0=m8[:, 0:1], in1=m8[:, 1:2], op=ALU.subtract), r=[m8], w=[dd])
                        K.A(lambda: nc.scalar.activation(out=dd[:], in_=dd[:], func=AF.Sigmoid), r=[dd], w=[dd])
                        K.V(lambda: nc.vector.tensor_tensor(out=wts[:, n, 0:1], in0=dd[:], in1=gw[:], op=ALU.mult), r=[dd, gw], w=[wts])
                        K.V(lambda: nc.vector.tensor_tensor(out=wts[:, n, 1:2], in0=gw[:], in1=wts[:, n, 0:1], op=ALU.subtract), r=[gw, wts], w=[wts])
                        K.P(lambda: nc.tensor.matmul(pc[:, 0, :], lhsT=trib[:], rhs=mk12[:], start=True, stop=True), r=[trib, mk12], w=[pcr], inc=False)
                        K.P(lambda: nc.tensor.matmul(pc[:, 1, :], lhsT=onesb[:], rhs=mk12[:], start=True, stop=True), r=[onesb, mk12], w=[pcr])
                        K.V(lambda: nc.vector.tensor_tensor(out=posv[:], in0=pc[:, 0, :], in1=run[:], op=ALU.add), r=[pcr, run], w=[posv])
                        K.V(lambda: nc.vector.tensor_tensor(out=posv[:], in0=posv[:], in1=cst[:, C_EB:C_EB + 32], op=ALU.add), r=[posv, cst], w=[posv])
                        K.V(lambda: nc.vector.tensor_tensor(out=run[:], in0=run[:], in1=pc[:, 1, :], op=ALU.add), r=[run, pcr], w=[run])
                        K.V(lambda: nc.vector.tensor_tensor(out=junk[:], in0=mk1[:], in1=posv[:], op=ALU.mult), r=[mk1, posv], w=[junk])
                        K.V(lambda: nc.vector.reduce_sum(out=sf[:, 0:1], in_=junk[:], axis=AX.X), r=[junk], w=[sf])
                        K.V(lambda: nc.vector.tensor_tensor(out=junk[:], in0=mk2[:], in1=posv[:], op=ALU.mult), r=[mk2, posv], w=[junk])
                        K.V(lambda: nc.vector.reduce_sum(out=sf[:, 1:2], in_=junk[:], axis=AX.X), r=[junk], w=[sf])
                        K.V(lambda: nc.vector.tensor_scalar(out=sf[:], in0=sf[:], scalar1=float(NSLOT - 1), scalar2=None, op0=ALU.min), r=[sf], w=[sf])
                        K.V(lambda: nc.vector.tensor_copy(out=slots[:, n, :], in_=sf[:]), r=[sf], w=[slots])
                        for j in range(2):
                            K.idma(xslot[:, :], rw[:, :], bass.IndirectOffsetOnAxis(ap=slots[:, n, j:j + 1], axis=0), None,
                                   r=[rw, slots], dres=rw)

            chk("P6")
            with K.phase():
                SG = 512
                NSG = CAP // SG
                NST = SG // 128
                wgs = [K.sb(f"ewg{i}", [128, 8, 512], BF16) for i in range(2)]
                wus = [K.sb(f"ewu{i}", [128, 8, 512], BF16) for i in range(2)]
                wds = [K.sb(f"ewd{i}", [128, 4, 1024], BF16) for i in range(2)]
                xrs = [K.sb(f"xr{i}", [128, NST, 1024], BF16) for i in range(2)]
                xTs = [K.sb(f"xT{i}", [128, 8, SG], BF16) for i in range(2)]
                hTs_ = [K.sb(f"hTe{i}", [128, 4, SG], BF16) for i in range(2)]
                sacts = [K.sb(f"sact{i}", [128, SG], F32) for i in range(2)]
                sai = 0
                yss = [K.sb(f"ys{i}", [128, 1024], F32) for i in range(2)]
                pb = K.ps("pb", [128, 6, 512], F32)
                pres = [K.res() for _ in range(6)]
                pst = K.ps("pst", [128, 2, 8, 128], BF16)
                pstr = [K.res(), K.res()]
                bki = [0]

                def nb():
                    bki[0] = (bki[0] + 1) % 6
                    return bki[0]
                yi = 0
                pi = 0
                xi = 0
                for e in range(32):
                    s = e % 2
                    wg, wu, wd = wgs[s], wus[s], wds[s]
                    K.dma(K.pool, wg[:], D["e_wg"][l, e].rearrange("(k p) n -> p k n", p=128), w=[wg], dres=wg)
                    K.dma(K.pool, wu[:], D["e_wu"][l, e].rearrange("(k p) n -> p k n", p=128), w=[wu], dres=wu)
                    K.dma(K.pool, wd[:], D["e_wd"][l, e].rearrange("(k p) n -> p k n", p=128), w=[wd], dres=wd)
                    for sgi in range(NSG):
                        base = e * CAP + sgi * SG
                        xr = xrs[xi % 2]
                        xT = xTs[xi % 2]
                        hT_ = hTs_[xi % 2]
                        if xi == 0:
                            K.dma(K.sp, xr[:], xslot[base:base + SG, :].rearrange("(a p) f -> p a f", p=128), w=[xr], dres=xr)
                        xi += 1
                        if xi < 32 * NSG:
                            nbase = (xi // NSG) * CAP + (xi % NSG) * SG
                            xrn = xrs[xi % 2]
                            K.dma(K.sp, xrn[:], xslot[nbase:nbase + SG, :].rearrange("(a p) f -> p a f", p=128), w=[xrn], dres=xrn)
                        for a in range(NST):
                            q = pi % 2
                            pi += 1
                            transposes(lambda i: xr[:, a, i * 128:(i + 1) * 128], 8, pst[:, q], pstr[q], [xr])
                            K.V(lambda: nc.vector.tensor_copy(out=xT[:, :, a * 128:(a + 1) * 128], in_=pst[:, q]), r=[pstr[q]], w=[xT])
                        for jc in range(4):
                            b1 = nb()
                            for k in range(8):
                                K.P(lambda: nc.tensor.matmul(pb[:, b1, :SG], lhsT=wg[:, k, jc * 128:(jc + 1) * 128], rhs=xT[:, k, :],
                                                             start=(k == 0), stop=(k == 7)), r=[wg, xT], w=[pres[b1]], inc=(k == 7))
                            b2 = nb()
                            for k in range(8):
                                K.P(lambda: nc.tensor.matmul(pb[:, b2, :SG], lhsT=wu[:, k, jc * 128:(jc + 1) * 128], rhs=xT[:, k, :],
                                                             start=(k == 0), stop=(k == 7)), r=[wu, xT], w=[pres[b2]], inc=(k == 7))
                            sact = sacts[sai % 2]
                            sai += 1
                            K.A(lambda: nc.scalar.activation(out=sact[:], in_=pb[:, b1, :SG], func=AF.Silu), r=[pres[b1]], w=[sact])
                            K.V(lambda: nc.vector.tensor_tensor(out=hT_[:, jc, :], in0=sact[:], in1=pb[:, b2, :SG], op=ALU.mult),
                                r=[sact, pres[b2]], w=[hT_])
                        for a in range(NST):
                            ys = yss[yi % 2]
                            yi += 1
                            for nh in range(2):
                                b = nb()
                                for jc in range(4):
                                    K.P(lambda: nc.tensor.matmul(pb[:, b, :], lhsT=hT_[:, jc, a * 128:(a + 1) * 128], rhs=wd[:, jc, nh * 512:(nh + 1) * 512],
                                                                 start=(jc == 0), stop=(jc == 3)), r=[hT_, wd], w=[pres[b]], inc=(jc == 3))
                                K.A(lambda: nc.scalar.copy(out=ys[:, nh * 512:(nh + 1) * 512], in_=pb[:, b, :]), r=[pres[b]], w=[ys])
                            K.dma(K.sp, yslot[base + a * 128:base + (a + 1) * 128, :], ys[:], r=[ys], dres=ys)

            chk("P7")
            with K.phase():
                y1s = [K.sb(f"y1_{i}", [128, 1024], F32) for i in range(2)]
                y2s = [K.sb(f"y2_{i}", [128, 1024], F32) for i in range(2)]
                hts = [K.sb(f"ht{i}", [128, 8, 128], F32) for i in range(2)]
                ptr = K.ps("ptr", [128, 2, 8, 128], F32)
                ptrr = [K.res(), K.res()]
                def loads8(n_):
                    s_ = n_ % 2
                    K.idma(y1s[s_][:, :], yslot[:, :], None, bass.IndirectOffsetOnAxis(ap=slots[:, n_, 0:1], axis=0), r=[slots], w=[y1s[s_]], dres=y1s[s_])
                    K.idma(y2s[s_][:, :], yslot[:, :], None, bass.IndirectOffsetOnAxis(ap=slots[:, n_, 1:2], axis=0), r=[slots], w=[y2s[s_]], dres=y2s[s_])
                    K.dma(K.sp, hts[s_][:], fm(hT, n_ * 128, 128), w=[hts[s_]], dres=hts[s_])
                loads8(0)
                for n in range(NT):
                    t0 = n * 128
                    s = n % 2
                    v = 1 if n < 2 else 0
                    y1, y2, ht = y1s[s], y2s[s], hts[s]
                    if n + 1 < NT:
                        loads8(n + 1)
                    K.V(lambda: nc.vector.tensor_scalar(out=y1[:], in0=y1[:], scalar1=wts[:, n, 0:1], scalar2=None, op0=ALU.mult), r=[y1, wts], w=[y1])
                    K.V(lambda: nc.vector.scalar_tensor_tensor(out=y1[:], in0=y2[:], scalar=wts[:, n, 1:2], in1=y1[:], op0=ALU.mult, op1=ALU.add),
                        r=[y2, wts, y1], w=[y1])
                    for k in range(8):
                        K.P(lambda: nc.tensor.transpose(out=ptr[:, s, k, :], in_=y1[:, k * 128:(k + 1) * 128], identity=cst[:, C_ID:C_ID + 128]),
                            r=[y1, cst], w=[ptrr[s]], inc=(k == 7))
                    K.V(lambda: nc.vector.tensor_tensor(out=y2[:].rearrange("p (k t) -> p k t", t=128), in0=ptr[:, s],
                                                        in1=bc(modv(l, 5, v).unsqueeze(2), [128, 8, 128]), op=ALU.mult),
                        r=[ptrr[s], modT, y2], w=[y2])
                    K.V(lambda: nc.vector.tensor_tensor(out=ht[:], in0=ht[:], in1=y2[:].rearrange("p (k t) -> p k t", t=128), op=ALU.add),
                        r=[ht, y2], w=[ht])
                    K.dma(K.sp, fm(hT, t0, 128), ht[:], r=[ht], dres=ht)

            chk("P8")
          except _Stop:
            break

        with K.phase():
            hbs = [K.sb(f"hb{i}", [128, 8, 512], F32) for i in range(2)]
            sqb = K.sb("sqb", [128, 8, 512], BF16)
            tmps = [K.sb(f"tmp{i}", [128, 8, 512], F32) for i in range(2)]
            rt = K.sb("rt", [128, 512], F32)
            rstd = K.sb("rstd", [128, 512], F32)
            pb = K.ps("pb", [128, 2, 512], F32)
            pres = [K.res(), K.res()]
            for bi in range(1, 9):
                t0, bs = TB[bi]
                hb, tmp = hbs[bi % 2], tmps[bi % 2]
                K.dma(K.sp, hb[:], fm(hT if n_layers > 0 else D["h0T"], t0, bs), w=[hb], dres=hb)
                norm_block(hb, bs, sqb, tmp, rt, rstd, pb[:, bi % 2, :], pres[bi % 2])
                K.V(lambda: nc.vector.tensor_tensor(out=tmp[:], in0=tmp[:], in1=bc(gfT[:, :].unsqueeze(2), [128, 8, 512]), op=ALU.mult),
                    r=[tmp, gfT], w=[tmp])
                K.dma(K.sp, OUT.rearrange("(k p) t -> p k t", p=128)[:, :, t0 - 256:t0 - 256 + bs], tmp[:], r=[tmp], dres=tmp)
    return nc


def _consts():
    c = np.zeros((128, 1412), np.float32)
    j = np.arange(128)[:, None].astype(np.float32)
    i = np.arange(128)[None, :].astype(np.float32)
    c[:, 0:128] = np.eye(128, dtype=np.float32)
    c[:, 128:256] = np.maximum(i - j, 0)
    c[:, 256:384] = (j < i)
    c[:, 384:512] = np.maximum(j - i, 0)
    c[:, 512:640] = (j > i)
    c[:, 640:768] = 2.0 * (i == j)
    c[:, 768:896] = (j >= i)
    c[:, 896:1024] = (j <= i)
    p = np.arange(128, dtype=np.float32)
    c[:, 1024] = p
    c[:, 1025] = 127 - p
    c[:, 1026] = p + 1
    c[:, 1027] = 128 - p
    c[:, 1028:1156] = (j < i)
    c[:, 1156:1188] = (np.arange(32, dtype=np.float32) * CAP)[None, :]
    c[:, 1188:1316] = 1.0
    return c


def _rope_tables():
    out = np.zeros((NT, 128, 256), np.float32)
    out[:, :, 0:64] = 1.0
    out[:, :, 128:192] = 1.0
    pos = np.arange(SEQ, dtype=np.float32)
    inv_r = (10000.0 ** (-(np.arange(0, 64, 2, dtype=np.float32) / 64.0))).astype(np.float32)
    ang = (pos[:, None] * inv_r[None, :]).astype(np.float32)
    cr, sr = np.cos(ang), np.sin(ang)
    inv_a = (10000.0 ** (-(np.arange(0, 32, 2, dtype=np.float32) / 32.0))).astype(np.float32)
    row = np.floor(pos / 64.0).astype(np.float32)
    col = (pos - row * 64.0).astype(np.float32)
    ar = (row[:, None] * inv_a[None, :]).astype(np.float32)
    ac = (col[:, None] * inv_a[None, :]).astype(np.float32)
    lat = np.zeros((SEQ, 256), np.float32)
    lat[:, 0:32] = cr
    lat[:, 32:64] = cr
    lat[:, 64:96] = -sr
    lat[:, 96:128] = sr
    lat[:, 128:144] = np.cos(ar)
    lat[:, 144:160] = np.cos(ar)
    lat[:, 160:176] = np.cos(ac)
    lat[:, 176:192] = np.cos(ac)
    lat[:, 192:208] = -np.sin(ar)
    lat[:, 208:224] = np.sin(ar)
    lat[:, 224:240] = -np.sin(ac)
    lat[:, 240:256] = np.sin(ac)
    out[2:] = lat.reshape(32, 128, 256)
    return out


def _fmv(a):
    a = np.asarray(a, np.float32)
    lead = a.shape[:-1]
    r = a.reshape(lead + (8, 128))
    r = np.moveaxis(r, -1, 0)
    return np.ascontiguousarray(r)


def prepare_inputs(inp):
    f = lambda a: np.ascontiguousarray(np.asarray(a, np.float32))
    shared = {}
    shared["w_mod"] = f(inp["w_mod"])
    shared["b_modT"] = np.ascontiguousarray(np.asarray(inp["b_mod"], np.float32).reshape(DEPTH, 48, 128).transpose(2, 0, 1))
    shared["g1T"] = _fmv(inp["norm1_g"])
    shared["g2T"] = _fmv(inp["norm2_g"])
    shared["gfT"] = _fmv(inp["final_norm_g"])
    shared["w_in"] = f(inp["w_in"])
    cw = np.asarray(inp["lru_conv_w"], np.float32)
    shared["conv_wT"] = np.ascontiguousarray(cw.reshape(DEPTH, 4, 8, 128).transpose(3, 0, 2, 1))
    shared["conv_bT"] = _fmv(inp["lru_conv_b"])
    for nm, key in (("wa_bd", "lru_wa"), ("wx_bd", "lru_wx")):
        w = np.asarray(inp[key], np.float32)
        bd = np.zeros((DEPTH, 8, 128, 2, 128), np.float32)
        for c in range(8):
            for hh in range(2):
                bd[:, c, hh * 64:(hh + 1) * 64, :, hh * 64:(hh + 1) * 64] = w[:, :, 2 * c + hh].transpose(0, 2, 1, 3)
        shared[nm] = bd
    shared["lru_baT"] = _fmv(inp["lru_ba"])
    shared["lru_bxT"] = _fmv(inp["lru_bx"])
    shared["lru_lamT"] = _fmv(inp["lru_lambda"])
    rl = np.asarray(inp["ret_lambda"], np.float32)
    shared["ret_lam_rep"] = np.ascontiguousarray(np.broadcast_to(rl[None], (128, DEPTH, 2, 8)))
    rs = np.zeros((128, DEPTH, 2, 4), np.float32)
    for pr in range(4):
        rs[0:64, :, :, pr] = rl[None, :, :, 2 * pr]
        rs[64:128, :, :, pr] = rl[None, :, :, 2 * pr + 1]
    shared["ret_lam_S"] = rs
    shared["sink_rep"] = np.ascontiguousarray(np.broadcast_to(np.asarray(inp["attn_sink"], np.float32)[None], (128, DEPTH, 16)))
    shared["w_ba"] = f(inp["w_branch_a"])
    shared["w_bb"] = f(inp["w_branch_b"])
    shared["w_bc"] = f(inp["w_branch_c"])
    shared["w_out"] = f(inp["w_out"])
    wr = np.concatenate([np.asarray(inp["router_group_w"], np.float32), np.asarray(inp["router_expert_w"], np.float32)], axis=-1)
    shared["w_rt"] = np.ascontiguousarray(wr.reshape(DEPTH, 8, 128, 36).transpose(2, 0, 1, 3))
    br = np.concatenate([np.asarray(inp["router_group_b"], np.float32), np.asarray(inp["router_expert_b"], np.float32)], axis=-1)
    shared["b_rt"] = np.ascontiguousarray(np.broadcast_to(br[None], (128, DEPTH, 36)))
    shared["e_wg"] = f(inp["expert_w_gate"])
    shared["e_wu"] = f(inp["expert_w_up"])
    shared["e_wd"] = f(inp["expert_w_down"])
    shared["consts"] = _consts()
    shared["rope"] = _rope_tables()
    x = np.asarray(inp["x"], np.float32)
    ctx = np.asarray(inp["ctx"], np.float32)
    c = np.asarray(inp["c"], np.float32)
    cc = np.asarray(inp["c_ctx"], np.float32)
    in_maps = []
    for b in range(N_CORES):
        m = dict(shared)
        m["h0T"] = np.ascontiguousarray(np.concatenate([ctx[b], x[b]], axis=0).T)
        cT = np.stack([c[b].reshape(8, 128).T, cc.reshape(8, 128).T], axis=-1)
        m["cT"] = np.ascontiguousarray(cT.astype(np.float32))
        in_maps.append(m)
    return in_maps


_NC_CACHE = {}


def kernel(**inputs):
    in_maps = prepare_inputs(inputs)
    if "nc" not in _NC_CACHE:
        _NC_CACHE["nc"] = build_program()
    res = run_bass_kernel_spmd(_NC_CACHE["nc"], in_maps, core_ids=list(range(N_CORES)))
    out = np.stack([np.ascontiguousarray(r["outT"].T) for r in res.results], axis=0)
    return out.astype(np.float32)
```

```python
import numpy as np
from contextlib import ExitStack, contextmanager
import concourse.bass as bass
import concourse.mybir as mybir
from concourse.bass_utils import run_bass_kernel_spmd

F32 = mybir.dt.float32
BF16 = mybir.dt.bfloat16
I32 = mybir.dt.int32
AF = mybir.ActivationFunctionType
ALU = mybir.AluOpType
AX = mybir.AxisListType

DEPTH = 4
T = 4352
NT = 34
LC = 256
SEQ = 4096
TB = [(0, 256)] + [(256 + 512 * i, 512) for i in range(8)]
CAP = 1024
NSLOT = 32 * CAP
EPS = 1e-6
N_CORES = 8


class SC:
    def __init__(self, nc, name):
        self.sem = nc.alloc_semaphore(name)
        self.n = 0


class Res:
    def __init__(self, name=""):
        self.w = None
        self.rs = {}
        self.dsc = None
        self.name = name


class Tile(Res):
    def __init__(self, t, name):
        super().__init__(name)
        self.t = t

    def __getitem__(self, k):
        return self.t[k]


class Eng:
    def __init__(self, nc, e, name, selfsync=True):
        self.e = e
        self.sc = SC(nc, "eng_" + name)
        self.waited = {}
        self.selfsync = selfsync

    def wait(self, tok):
        sc, val = tok
        if sc is self.sc and not self.selfsync:
            return
        if self.waited.get(sc, 0) >= val:
            return
        self.waited[sc] = val
        self.e.wait_ge(sc.sem, val)


class Kern:
    def __init__(self, nc):
        self.nc = nc
        self.pe = Eng(nc, nc.tensor, "pe", selfsync=False)
        self.act = Eng(nc, nc.scalar, "act")
        self.dve = Eng(nc, nc.vector, "dve")
        self.pool = Eng(nc, nc.gpsimd, "pool")
        self.sp = Eng(nc, nc.sync, "sp")
        self.engs = [self.pe, self.act, self.dve, self.pool, self.sp]
        self.scs = [e.sc for e in self.engs]
        self.free_dsc = []
        self.ndsc = 0
        self.stacks = []
        self.uid = 0
        self.bank_i = 0

    @contextmanager
    def phase(self):
        st = ExitStack()
        self.stacks.append((st, []))
        try:
            yield
        finally:
            self.barrier()
            _, used = self.stacks.pop()
            for r in used:
                if r.dsc is not None:
                    self.free_dsc.append(r.dsc)
                    r.dsc = None
            st.close()

    def sb(self, name, shape, dt):
        self.uid += 1
        t = self.stacks[-1][0].enter_context(self.nc.sbuf_tensor(f"{name}_{self.uid}", list(shape), dt))
        r = Tile(t, name)
        self.stacks[-1][1].append(r)
        return r

    def ps(self, name, shape, dt):
        self.uid += 1
        t = self.stacks[-1][0].enter_context(self.nc.psum_tensor(f"{name}_{self.uid}", list(shape), dt))
        r = Tile(t, name)
        self.stacks[-1][1].append(r)
        return r

    def res(self, name=""):
        r = Res(name)
        self.stacks[-1][1].append(r)
        return r

    def get_dsc(self, r):
        if r.dsc is None:
            if self.free_dsc:
                r.dsc = self.free_dsc.pop()
            else:
                self.ndsc += 1
                r.dsc = SC(self.nc, f"dma{self.ndsc}")
                self.scs.append(r.dsc)
        return r.dsc

    def _pre(self, eng, reads, writes):
        for r in reads:
            if r.w is not None:
                eng.wait(r.w)
        for w in writes:
            if w.w is not None:
                eng.wait(w.w)
            for sc, val in w.rs.items():
                eng.wait((sc, val))

    def _post(self, tok, reads, writes):
        for r in reads:
            if r.rs.get(tok[0], 0) < tok[1]:
                r.rs[tok[0]] = tok[1]
        for w in writes:
            w.w = tok
            w.rs = {}

    def op(self, eng, fn, r=(), w=(), inc=True):
        self._pre(eng, r, w)
        ins = fn()
        if inc:
            eng.sc.n += 1
            ins.then_inc(eng.sc.sem, 1)
            tok = (eng.sc, eng.sc.n)
        else:
            tok = (eng.sc, eng.sc.n + 1)
        self._post(tok, r, w)
        return ins

    def V(self, fn, r=(), w=()):
        return self.op(self.dve, fn, r, w)

    def A(self, fn, r=(), w=()):
        return self.op(self.act, fn, r, w)

    def G(self, fn, r=(), w=()):
        return self.op(self.pool, fn, r, w)

    def P(self, fn, r=(), w=(), inc=True):
        return self.op(self.pe, fn, r, w, inc)

    def dma(self, q, out, in_, r=(), w=(), dres=None, **kw):
        self._pre(q, r, w)
        ins = q.e.dma_start(out=out, in_=in_, **kw)
        sc = self.get_dsc(dres)
        sc.n += 16
        ins.then_inc(sc.sem, 16)
        self._post((sc, sc.n), r, w)
        return ins

    def idma(self, out, in_, out_off, in_off, r=(), w=(), dres=None):
        q = self.pool
        self._pre(q, r, w)
        ins = q.e.indirect_dma_start(out=out, out_offset=out_off, in_=in_, in_offset=in_off)
        sc = self.get_dsc(dres)
        sc.n += 16
        ins.then_inc(sc.sem, 16)
        self._post((sc, sc.n), r, w)
        return ins

    def barrier(self):
        for e in self.engs:
            for sc in self.scs:
                if sc.n > 0:
                    e.wait((sc, sc.n))

    def bank(self):
        self.bank_i = (self.bank_i + 1) % 8
        return self.bank_i


def bc(ap, shape):
    return ap.to_broadcast(list(shape))


class _Stop(Exception):
    pass


def build_program(n_layers=DEPTH, dbg=None, stop=None):
    scr = {}

    def chk(p):
        if stop == p:
            if dbg is not None and dbg[0] in scr:
                scr["K"].barrier()
                scr["K"].dma(scr["K"].sp, scr["DBG"], scr[dbg[0]], dres=scr["cst"])
                scr["K"].barrier()
            raise _Stop()
    nc = bass.Bass("TRN2", target_bir_lowering=False)
    K = Kern(nc)

    def din(name, shape, dt=F32):
        return nc.dram_tensor(name, list(shape), dt, kind="ExternalInput").ap()

    def dscr(name, shape, dt):
        return nc.dram_tensor(name, list(shape), dt, kind="Internal").ap()

    D = {}
    D["h0T"] = din("h0T", [1024, T])
    D["cT"] = din("cT", [128, 8, 2])
    D["w_mod"] = din("w_mod", [DEPTH, 1024, 6144])
    D["b_modT"] = din("b_modT", [128, DEPTH, 48])
    D["g1T"] = din("g1T", [128, DEPTH, 8])
    D["g2T"] = din("g2T", [128, DEPTH, 8])
    D["gfT"] = din("gfT", [128, 8])
    D["w_in"] = din("w_in", [DEPTH, 1024, 9728])
    D["conv_wT"] = din("conv_wT", [128, DEPTH, 8, 4])
    D["conv_bT"] = din("conv_bT", [128, DEPTH, 8])
    D["wa_bd"] = din("wa_bd", [DEPTH, 8, 128, 2, 128])
    D["wx_bd"] = din("wx_bd", [DEPTH, 8, 128, 2, 128])
    D["lru_baT"] = din("lru_baT", [128, DEPTH, 2, 8])
    D["lru_bxT"] = din("lru_bxT", [128, DEPTH, 2, 8])
    D["lru_lamT"] = din("lru_lamT", [128, DEPTH, 2, 8])
    D["ret_lam_rep"] = din("ret_lam_rep", [128, DEPTH, 2, 8])
    D["ret_lam_S"] = din("ret_lam_S", [128, DEPTH, 2, 4])
    D["sink_rep"] = din("sink_rep", [128, DEPTH, 16])
    D["w_ba"] = din("w_ba", [DEPTH, 1024, 1024])
    D["w_bb"] = din("w_bb", [DEPTH, 1024, 1024])
    D["w_bc"] = din("w_bc", [DEPTH, 1024, 1024])
    D["w_out"] = din("w_out", [DEPTH, 1024, 1024])
    D["w_rt"] = din("w_rt", [128, DEPTH, 8, 36])
    D["b_rt"] = din("b_rt", [128, DEPTH, 36])
    D["e_wg"] = din("e_wg", [DEPTH, 32, 1024, 512])
    D["e_wu"] = din("e_wu", [DEPTH, 32, 1024, 512])
    D["e_wd"] = din("e_wd", [DEPTH, 32, 512, 1024])
    D["consts"] = din("consts", [128, 1412])
    D["rope"] = din("rope", [NT, 128, 256])

    OUT = nc.dram_tensor("outT", [1024, SEQ], F32, kind="ExternalOutput").ap()
    DBG = None
    if dbg is not None:
        DBG = nc.dram_tensor("dbg", list(dbg[1]), dbg[2], kind="ExternalOutput").ap()

    hT = dscr("hT", [1024, T], F32)
    yaT = dscr("yaT", [1024, T], BF16)
    ybT = dscr("ybT", [1024, T], BF16)
    ycT = dscr("ycT", [1024, T], BF16)
    sgT = dscr("sgT", [3072, T], BF16)
    rqT = dscr("rqT", [512, T], BF16)
    rkT = dscr("rkT", [512, T], BF16)
    rk = dscr("rk", [T, 512], BF16)
    rv = dscr("rv", [T, 1024], BF16)
    rg = dscr("rg", [T, 1024], BF16)
    aqT = dscr("aqT", [1024, T], BF16)
    akT = dscr("akT", [256, T], BF16)
    av = dscr("av", [T, 256], BF16)
    SfD = dscr("SfD", [NT, 128, 512], BF16)
    xslot = dscr("xslot", [NSLOT, 1024], BF16)
    yslot = dscr("yslot", [NSLOT, 1024], BF16)

    scr.update(dict(K=K, DBG=DBG, hT=hT, yaT=yaT, ybT=ybT, ycT=ycT, sgT=sgT, rqT=rqT, rkT=rkT, rk=rk, rv=rv, rg=rg,
                    aqT=aqT, akT=akT, av=av, xslot=xslot, yslot=yslot))

    def fm(ap2d, t0, bs):
        return ap2d.rearrange("(k p) t -> p k t", p=128)[:, :, t0:t0 + bs]

    with K.phase():
        cst = K.sb("cst", [128, 1412], F32)
        scr["cst"] = cst
        K.dma(K.sp, cst[:], D["consts"], w=[cst], dres=cst)
        C_ID, C_R1, C_M1, C_R2, C_M2, C_I2, C_MP, C_MN = [i * 128 for i in range(8)]
        C_CV = 1024
        C_TRI = 1028
        C_EB = 1156
        C_ONE = 1188
        identb = K.sb("identb", [128, 128], BF16)
        identf = cst
        onesb = K.sb("onesb", [128, 128], BF16)
        maskp = K.sb("maskp", [128, 128], BF16)
        maskn = K.sb("maskn", [128, 128], BF16)
        trib = K.sb("trib", [128, 128], BF16)
        epsT = K.sb("epsT", [128, 1], F32)
        K.V(lambda: nc.vector.tensor_copy(out=identb[:], in_=cst[:, C_ID:C_ID + 128]), r=[cst], w=[identb])
        K.V(lambda: nc.vector.tensor_copy(out=onesb[:], in_=cst[:, C_ONE:C_ONE + 128]), r=[cst], w=[onesb])
        K.V(lambda: nc.vector.tensor_copy(out=maskp[:], in_=cst[:, C_MP:C_MP + 128]), r=[cst], w=[maskp])
        K.V(lambda: nc.vector.tensor_copy(out=maskn[:], in_=cst[:, C_MN:C_MN + 128]), r=[cst], w=[maskn])
        K.V(lambda: nc.vector.tensor_copy(out=trib[:], in_=cst[:, C_TRI:C_TRI + 128]), r=[cst], w=[trib])
        K.V(lambda: nc.vector.memset(epsT[:], EPS), w=[epsT])
        modT = K.sb("modT", [128, DEPTH, 48, 2], F32)
        g1T = K.sb("g1T", [128, DEPTH, 8], F32)
        g2T = K.sb("g2T", [128, DEPTH, 8], F32)
        gfT = K.sb("gfT", [128, 8], F32)
        K.dma(K.sp, g1T[:], D["g1T"], w=[g1T], dres=g1T)
        K.dma(K.sp, g2T[:], D["g2T"], w=[g2T], dres=g2T)
        K.dma(K.sp, gfT[:], D["gfT"], w=[gfT], dres=gfT)
        gs1 = K.sb("gs1", [128, 8, 2], F32)
        gs2 = K.sb("gs2", [128, 8, 2], F32)
        slots = K.sb("slots", [128, NT, 2], I32)
        wts = K.sb("wts", [128, NT, 2], F32)

        def modv(l, m, v):
            return modT[:, l, m * 8:(m + 1) * 8, v]

        def mods(l, m, k, v):
            return modT[:, l, m * 8 + k, v:v + 1]

        with K.phase():
            zt = K.sb("zt", [128, 8192], BF16)
            K.G(lambda: nc.gpsimd.memset(zt[:], 0.0), w=[zt])
            xz = xslot.rearrange("(p a) f -> p (a f)", p=128)
            for zi in range(NSLOT * 1024 // (128 * 8192)):
                K.dma(K.sp, xz[:, zi * 8192:(zi + 1) * 8192], zt[:], r=[zt], dres=zt)
            cT = K.sb("cT", [128, 8, 2], F32)
            sgm = K.sb("sgm", [128, 8, 2], F32)
            sT = K.sb("sT", [128, 8, 2], F32)
            bm = K.sb("bm", [128, DEPTH, 48], F32)
            K.dma(K.sp, cT[:], D["cT"], w=[cT], dres=cT)
            K.dma(K.sp, bm[:], D["b_modT"], w=[bm], dres=bm)
            K.A(lambda: nc.scalar.activation(out=sgm[:], in_=cT[:], func=AF.Sigmoid), r=[cT], w=[sgm])
            K.V(lambda: nc.vector.tensor_tensor(out=sT[:], in0=cT[:], in1=sgm[:], op=ALU.mult), r=[cT, sgm], w=[sT])
            wm = [K.sb(f"wm{i}", [128, 8, 512], F32) for i in range(6)]
            pm = K.ps("pm", [128, 48, 2], F32)
            it = 0
            for l in range(n_layers):
                for jj in range(12):
                    wt = wm[it % 6]
                    it += 1
                    K.dma(K.sp, wt[:], D["w_mod"][l, :, jj * 512:(jj + 1) * 512].rearrange("(k p) n -> p k n", p=128),
                          w=[wt], dres=wt)
                    for j4 in range(4):
                        j = jj * 4 + j4
                        for k in range(8):
                            K.P(lambda: nc.tensor.matmul(pm[:, j, :], lhsT=wt[:, k, j4 * 128:(j4 + 1) * 128], rhs=sT[:, k, :],
                                                         start=(k == 0), stop=(k == 7)),
                                r=[wt, sT], w=[pm], inc=(k == 7))
                K.V(lambda: nc.vector.tensor_tensor(out=modT[:, l], in0=pm[:], in1=bc(bm[:, l, :].unsqueeze(2), [128, 48, 2]),
                                                    op=ALU.add), r=[pm, bm], w=[modT])

        def norm_block(hb, bs, sqb, tmp, rt, rstd, pbank, pres):
            K.A(lambda: nc.scalar.activation(out=sqb[:, :, :bs], in_=hb[:, :, :bs], func=AF.Square), r=[hb], w=[sqb])
            for k in range(8):
                K.P(lambda: nc.tensor.matmul(pbank[:, :bs], lhsT=onesb[:], rhs=sqb[:, k, :bs], start=(k == 0), stop=(k == 7)),
                    r=[onesb, sqb], w=[pres], inc=(k == 7))
            K.A(lambda: nc.scalar.activation(out=rt[:, :bs], in_=pbank[:, :bs], func=AF.Sqrt, bias=epsT[:, 0:1], scale=1.0 / 1024.0),
                r=[pres, epsT], w=[rt])
            K.V(lambda: nc.vector.reciprocal(out=rstd[:, :bs], in_=rt[:, :bs]), r=[rt], w=[rstd])
            K.V(lambda: nc.vector.tensor_tensor(out=tmp[:, :, :bs], in0=hb[:, :, :bs],
                                                in1=bc(rstd[:, :bs].unsqueeze(1), [128, 8, bs]), op=ALU.mult),
                r=[hb, rstd], w=[tmp])

        def transposes(src_ap_fn, n, pst, pst_res, srcs):
            for i in range(n):
                K.P(lambda: nc.tensor.transpose(out=pst[:, i, :], in_=src_ap_fn(i), identity=identb[:]),
                    r=list(srcs) + [identb], w=[pst_res], inc=(i == n - 1))

        for l in range(n_layers):
          try:
            src_h = D["h0T"] if l == 0 else hT
            for v in range(2):
                K.V(lambda: nc.vector.scalar_tensor_tensor(out=gs1[:, :, v], in0=modv(l, 1, v), scalar=1.0, in1=g1T[:, l, :],
                                                           op0=ALU.add, op1=ALU.mult), r=[modT, g1T], w=[gs1])
                K.V(lambda: nc.vector.scalar_tensor_tensor(out=gs2[:, :, v], in0=modv(l, 4, v), scalar=1.0, in1=g2T[:, l, :],
                                                           op0=ALU.add, op1=ALU.mult), r=[modT, g2T], w=[gs2])

            with K.phase():
                uT = K.sb("uT", [128, 8, T], BF16)
                with K.phase():
                    hbs = [K.sb(f"hb{i}", [128, 8, 512], F32) for i in range(2)]
                    sqb = K.sb("sqb", [128, 8, 512], BF16)
                    tmp = K.sb("tmp", [128, 8, 512], F32)
                    rt = K.sb("rt", [128, 512], F32)
                    rstd = K.sb("rstd", [128, 512], F32)
                    pb = K.ps("pb", [128, 2, 512], F32)
                    pres = [K.res(), K.res()]
                    for bi, (t0, bs) in enumerate(TB):
                        hb = hbs[bi % 2]
                        v = 1 if bi == 0 else 0
                        K.dma(K.sp, hb[:, :, :bs], fm(src_h, t0, bs), w=[hb], dres=hb)
                        norm_block(hb, bs, sqb, tmp, rt, rstd, pb[:, bi % 2, :], pres[bi % 2])
                        for k in range(8):
                            K.A(lambda: nc.scalar.activation(out=uT[:, k, t0:t0 + bs], in_=tmp[:, k, :bs], func=AF.Identity,
                                                             bias=mods(l, 0, k, v), scale=gs1[:, k, v:v + 1]),
                                r=[tmp, gs1, modT], w=[uT])
                        if l == 0:
                            K.dma(K.sp, fm(hT, t0, bs), hb[:, :, :bs], r=[hb], dres=hb)
                if dbg is not None and dbg[0] == "uT" and l == dbg[3]:
                    K.dma(K.sp, DBG.rearrange("(k p) t -> p k t", p=128), uT[:], r=[uT], dres=uT)
                chk("P1")

                with K.phase():
                    lam = K.sb("lam", [128, 2, 8], F32)
                    ex = K.sb("ex", [128, 2, 8], F32)
                    sp1 = K.sb("sp1", [128, 2, 8], F32)
                    sp2 = K.sb("sp2", [128, 2, 8], F32)
                    ba = K.sb("ba", [128, 2, 8], F32)
                    bx = K.sb("bx", [128, 2, 8], F32)
                    cw = K.sb("cw", [128, 8, 4], F32)
                    cb = K.sb("cb", [128, 8], F32)
                    K.dma(K.sp, lam[:], D["lru_lamT"][:, l], w=[lam], dres=lam)
                    K.dma(K.sp, ba[:], D["lru_baT"][:, l], w=[ba], dres=ba)
                    K.dma(K.sp, bx[:], D["lru_bxT"][:, l], w=[bx], dres=bx)
                    K.dma(K.sp, cw[:], D["conv_wT"][:, l], w=[cw], dres=cw)
                    K.dma(K.sp, cb[:], D["conv_bT"][:, l], w=[cb], dres=cb)
                    K.A(lambda: nc.scalar.activation(out=ex[:], in_=lam[:], func=AF.Exp, scale=-1.0), r=[lam], w=[ex])
                    K.A(lambda: nc.scalar.activation(out=ex[:], in_=ex[:], func=AF.Ln, bias=1.0), r=[ex], w=[ex])
                    K.V(lambda: nc.vector.tensor_scalar(out=sp1[:], in0=ex[:], scalar1=-8.0, scalar2=None, op0=ALU.mult), r=[ex], w=[sp1])
                    K.V(lambda: nc.vector.tensor_scalar(out=sp2[:], in0=ex[:], scalar1=-16.0, scalar2=None, op0=ALU.mult), r=[ex], w=[sp2])
                    xa = K.sb("xa", [128, T + 8], F32)
                    uu = K.sb("uu", [128, T], F32)
                    ubs = [K.sb(f"ub{i}", [128, T], BF16) for i in range(2)]
                    hs = K.sb("hs", [128, T], F32)
                    rr_l = [K.sb(f"rr{i}", [128, 2048], F32) for i in range(2)]
                    ii_l = [K.sb(f"ii{i}", [128, 2048], F32) for i in range(2)]
                    a2_l = [K.sb(f"a2{i}", [128, 2048], F32) for i in range(2)]
                    sgi_ = [0]
                    stt = K.sb("stt", [128, 1], F32)
                    wxs = [K.sb(f"wxs{i}", [128, 8, 128], BF16) for i in range(2)]
                    wys = [K.sb(f"wys{i}", [128, 8, 128], BF16) for i in range(2)]
                    bdAs = [K.sb(f"bdA{i}", [128, 2, 128], BF16) for i in range(2)]
                    bdXs = [K.sb(f"bdX{i}", [128, 2, 128], BF16) for i in range(2)]
                    gy = K.sb("gy", [128, 512], BF16)
                    yos = [K.sb(f"yo{i}", [128, 512], BF16) for i in range(2)]
                    pb = K.ps("pb", [128, 8, 512], F32)
                    pres = [K.res() for _ in range(8)]
                    K.G(lambda: nc.gpsimd.memset(xa[:], 0.0), w=[xa])
                    win = D["w_in"][l].rearrange("(k p) n -> p k n", p=128)

                    def xoff(t):
                        return t + 2 if t < 256 else t + 5
                    yoi = [0]

                    def startup(c):
                        wx_, wy_, bdA, bdX, ub = wxs[c % 2], wys[c % 2], bdAs[c % 2], bdXs[c % 2], ubs[c % 2]
                        K.dma(K.pool, wx_[:], win[:, :, c * 128:(c + 1) * 128], w=[wx_], dres=wx_)
                        K.dma(K.pool, wy_[:], win[:, :, 1024 + c * 128:1024 + (c + 1) * 128], w=[wy_], dres=wy_)
                        K.dma(K.pool, bdA[:], D["wa_bd"][l, c], w=[bdA], dres=bdA)
                        K.dma(K.pool, bdX[:], D["wx_bd"][l, c], w=[bdX], dres=bdX)
                        for bi, (t0, bs) in enumerate(TB):
                            b = K.bank()
                            for k in range(8):
                                K.P(lambda: nc.tensor.matmul(pb[:, b, :bs], lhsT=wx_[:, k, :], rhs=uT[:, k, t0:t0 + bs],
                                                             start=(k == 0), stop=(k == 7)), r=[wx_, uT], w=[pres[b]], inc=(k == 7))
                            K.A(lambda: nc.scalar.copy(out=xa[:, xoff(t0):xoff(t0) + bs], in_=pb[:, b, :bs]), r=[pres[b]], w=[xa])
                        for (s0, n, po) in [(0, 256, 0), (256, 4096, 259)]:
                            K.V(lambda: nc.vector.tensor_scalar(out=uu[:, s0:s0 + n], in0=xa[:, po:po + n], scalar1=cw[:, c, 0:1],
                                                                scalar2=cb[:, c:c + 1], op0=ALU.mult, op1=ALU.add),
                                r=[xa, cw, cb], w=[uu])
                            for j in range(1, 3):
                                K.V(lambda: nc.vector.scalar_tensor_tensor(out=uu[:, s0:s0 + n], in0=xa[:, po + j:po + j + n],
                                                                           scalar=cw[:, c, j:j + 1], in1=uu[:, s0:s0 + n],
                                                                           op0=ALU.mult, op1=ALU.add), r=[xa, cw, uu], w=[uu])
                            K.V(lambda: nc.vector.scalar_tensor_tensor(out=ub[:, s0:s0 + n], in0=xa[:, po + 3:po + 3 + n],
                                                                       scalar=cw[:, c, 3:4], in1=uu[:, s0:s0 + n],
                                                                       op0=ALU.mult, op1=ALU.add), r=[xa, cw, uu], w=[ub])

                    def mainp(c):
                        wx_, wy_, bdA, bdX, ub = wxs[c % 2], wys[c % 2], bdAs[c % 2], bdXs[c % 2], ubs[c % 2]
                        segs = [(0, 256), (256, 2048), (2304, 2048)]
                        for d in range(2):
                            order = segs if d == 0 else [segs[0], segs[2], segs[1]]
                            for si, (s0, n) in enumerate(order):
                                rr, ii, a2 = rr_l[sgi_[0] % 2], ii_l[sgi_[0] % 2], a2_l[sgi_[0] % 2]
                                sgi_[0] += 1
                                for off in range(0, n, 512):
                                    bs = min(512, n - off)
                                    b1 = K.bank()
                                    K.P(lambda: nc.tensor.matmul(pb[:, b1, :bs], lhsT=bdA[:, d, :], rhs=ub[:, s0 + off:s0 + off + bs],
                                                                 start=True, stop=True), r=[bdA, ub], w=[pres[b1]])
                                    K.A(lambda: nc.scalar.activation(out=rr[:, off:off + bs], in_=pb[:, b1, :bs], func=AF.Sigmoid,
                                                                     bias=ba[:, d, c:c + 1]), r=[pres[b1], ba], w=[rr])
                                    b2 = K.bank()
                                    K.P(lambda: nc.tensor.matmul(pb[:, b2, :bs], lhsT=bdX[:, d, :], rhs=ub[:, s0 + off:s0 + off + bs],
                                                                 start=True, stop=True), r=[bdX, ub], w=[pres[b2]])
                                    K.A(lambda: nc.scalar.activation(out=ii[:, off:off + bs], in_=pb[:, b2, :bs], func=AF.Sigmoid,
                                                                     bias=bx[:, d, c:c + 1]), r=[pres[b2], bx], w=[ii])
                                K.A(lambda: nc.scalar.activation(out=a2[:, :n], in_=rr[:, :n], func=AF.Exp, scale=sp2[:, d, c:c + 1]),
                                    r=[rr, sp2], w=[a2])
                                K.A(lambda: nc.scalar.activation(out=rr[:, :n], in_=rr[:, :n], func=AF.Exp, scale=sp1[:, d, c:c + 1]),
                                    r=[rr, sp1], w=[rr])
                                K.A(lambda: nc.scalar.activation(out=a2[:, :n], in_=a2[:, :n], func=AF.Sqrt, bias=1.0, scale=-1.0),
                                    r=[a2], w=[a2])
                                K.V(lambda: nc.vector.tensor_tensor(out=ii[:, :n], in0=ii[:, :n], in1=ub[:, s0:s0 + n], op=ALU.mult),
                                    r=[ii, ub], w=[ii])
                                K.V(lambda: nc.vector.tensor_tensor(out=ii[:, :n], in0=ii[:, :n], in1=a2[:, :n], op=ALU.mult),
                                    r=[ii, a2], w=[ii])
                                if d == 0:
                                    init = 0.0 if si == 0 else hs[:, s0 - 1:s0]
                                    K.V(lambda: nc.vector.tensor_tensor_scan(out=hs[:, s0:s0 + n], data0=rr[:, :n], data1=ii[:, :n],
                                                                             initial=init, op0=ALU.mult, op1=ALU.add),
                                        r=[rr, ii, hs], w=[hs])
                                else:
                                    init = 0.0 if si == 0 else stt[:, 0:1]
                                    K.V(lambda: nc.vector.tensor_tensor_scan(out=a2[:, 0:n][:, ::-1], data0=rr[:, 0:n][:, ::-1],
                                                                             data1=ii[:, 0:n][:, ::-1], initial=init,
                                                                             op0=ALU.mult, op1=ALU.add),
                                        r=[rr, ii, stt], w=[a2])
                                    K.V(lambda: nc.vector.tensor_copy(out=stt[:], in_=a2[:, 0:1]), r=[a2], w=[stt])
                                    K.G(lambda: nc.gpsimd.tensor_tensor(out=hs[:, s0:s0 + n], in0=hs[:, s0:s0 + n], in1=a2[:, :n], op=ALU.add),
                                        r=[hs, a2], w=[hs])

                    def tailp(c):
                        wx_, wy_, bdA, bdX, ub = wxs[c % 2], wys[c % 2], bdAs[c % 2], bdXs[c % 2], ubs[c % 2]
                        for bi, (t0, bs) in enumerate(TB):
                            b = K.bank()
                            for k in range(8):
                                K.P(lambda: nc.tensor.matmul(pb[:, b, :bs], lhsT=wy_[:, k, :], rhs=uT[:, k, t0:t0 + bs],
                                                             start=(k == 0), stop=(k == 7)), r=[wy_, uT], w=[pres[b]], inc=(k == 7))
                            K.A(lambda: nc.scalar.activation(out=gy[:, :bs], in_=pb[:, b, :bs], func=AF.Gelu_apprx_tanh), r=[pres[b]], w=[gy])
                            yo = yos[yoi[0] % 2]
                            yoi[0] += 1
                            K.V(lambda: nc.vector.tensor_tensor(out=yo[:, :bs], in0=gy[:, :bs], in1=hs[:, t0:t0 + bs], op=ALU.mult),
                                r=[gy, hs], w=[yo])
                            K.dma(K.sp, yaT[c * 128:(c + 1) * 128, t0:t0 + bs], yo[:, :bs], r=[yo], dres=yo)


                    startup(0)
                    for c in range(8):
                        if c + 1 < 8:
                            startup(c + 1)
                        mainp(c)
                        tailp(c)

                chk("P2a")
                with K.phase():
                    wgs = [K.sb(f"wg{i}", [128, 8, 128], BF16) for i in range(2)]
                    obs = [K.sb(f"ob{i}", [128, T], BF16) for i in range(2)]
                    pb = K.ps("pb", [128, 8, 512], F32)
                    pres = [K.res() for _ in range(8)]
                    win = D["w_in"][l].rearrange("(k p) n -> p k n", p=128)
                    for cc in range(24):
                        wg, ob = wgs[cc % 2], obs[cc % 2]
                        K.dma(K.pool, wg[:], win[:, :, 6656 + cc * 128:6656 + (cc + 1) * 128], w=[wg], dres=wg)
                        for bi, (t0, bs) in enumerate(TB):
                            b = K.bank()
                            for k in range(8):
                                K.P(lambda: nc.tensor.matmul(pb[:, b, :bs], lhsT=wg[:, k, :], rhs=uT[:, k, t0:t0 + bs],
                                                             start=(k == 0), stop=(k == 7)), r=[wg, uT], w=[pres[b]], inc=(k == 7))
                            K.A(lambda: nc.scalar.activation(out=ob[:, t0:t0 + bs], in_=pb[:, b, :bs], func=AF.Sigmoid), r=[pres[b]], w=[ob])
                        K.dma(K.sp, sgT[cc * 128:(cc + 1) * 128, :], ob[:], r=[ob], dres=ob)

                chk("P2b")
                with K.phase():
                    Wt = K.sb("Wt", [128, 8, 4608], BF16)
                    win = D["w_in"][l].rearrange("(k p) n -> p k n", p=128)
                    for g in range(9):
                        K.dma(K.pool, Wt[:, :, g * 512:(g + 1) * 512], win[:, :, 2048 + g * 512:2048 + (g + 1) * 512], w=[Wt], dres=Wt)
                    ropes = [K.sb(f"rope{i}", [128, 256], F32) for i in range(2)]
                    xs_l = [K.sb(f"xs{i}", [128, 512], F32) for i in range(3)]
                    xsw_l = [K.sb(f"xsw{i}", [128, 512], F32) for i in range(3)]
                    t1_l = [K.sb(f"t1{i}", [128, 512], F32) for i in range(3)]
                    t2_l = [K.sb(f"t2{i}", [128, 512], F32) for i in range(3)]
                    rpi = [0]
                    NB = 2
                    o_q = [K.sb(f"oq{i}", [128, 512], BF16) for i in range(NB)]
                    o_k = [K.sb(f"ok{i}", [128, 512], BF16) for i in range(NB)]
                    o_v = [K.sb(f"ov{i}", [128, 1024], BF16) for i in range(NB)]
                    o_g = [K.sb(f"og{i}", [128, 1024], BF16) for i in range(NB)]
                    o_cq = [K.sb(f"ocq{i}", [128, 1024], BF16) for i in range(NB)]
                    o_ck = [K.sb(f"ock{i}", [128, 256], BF16) for i in range(NB)]
                    o_cv = [K.sb(f"ocv{i}", [128, 256], BF16) for i in range(NB)]
                    tq = [K.sb(f"tq{i}", [128, 4, 128], BF16) for i in range(NB)]
                    tk = [K.sb(f"tk{i}", [128, 4, 128], BF16) for i in range(NB)]
                    tcq = [K.sb(f"tcq{i}", [128, 8, 128], BF16) for i in range(NB)]
                    tck = [K.sb(f"tck{i}", [128, 2, 128], BF16) for i in range(NB)]
                    pb = K.ps("pb", [128, 6, 512], F32)
                    pres = [K.res() for _ in range(6)]
                    pst = K.ps("pst", [128, 2, 8, 128], BF16)
                    pstr = [K.res(), K.res()]
                    bki = [0]
                    psti = [0]

                    def nb():
                        bki[0] = (bki[0] + 1) % 6
                        return bki[0]

                    def proj(n, g):
                        b = nb()
                        for k in range(8):
                            K.P(lambda: nc.tensor.matmul(pb[:, b, :], lhsT=uT[:, k, n * 128:(n + 1) * 128], rhs=Wt[:, k, g * 512:(g + 1) * 512],
                                                         start=(k == 0), stop=(k == 7)), r=[uT, Wt], w=[pres[b]], inc=(k == 7))
                        pcount[0] += 1
                        flush()
                        return b

                    def rope(b, ncol, hsz, cos_ap, sin_ap, scale, out_t, ooff, ropet):
                        G = ncol // (2 * hsz)
                        H = ncol // 64
                        xs, xsw, t1, t2 = xs_l[rpi[0] % 3], xsw_l[rpi[0] % 3], t1_l[rpi[0] % 3], t2_l[rpi[0] % 3]
                        rpi[0] += 1
                        K.A(lambda: nc.scalar.activation(out=xs[:, :ncol], in_=pb[:, b, :ncol], func=AF.Copy, scale=scale), r=[pres[b]], w=[xs])
                        pv = pb[:, b, :ncol].rearrange("p (g two x) -> p g two x", two=2, x=hsz)
                        xv = xsw[:, :ncol].rearrange("p (g two x) -> p g two x", two=2, x=hsz)
                        K.A(lambda: nc.scalar.activation(out=xv[:, :, 0, :], in_=pv[:, :, 1, :], func=AF.Copy, scale=scale), r=[pres[b]], w=[xsw])
                        K.A(lambda: nc.scalar.activation(out=xv[:, :, 1, :], in_=pv[:, :, 0, :], func=AF.Copy, scale=scale), r=[pres[b]], w=[xsw])
                        x3 = xs[:, :ncol].rearrange("p (h x) -> p h x", x=64)
                        w3 = xsw[:, :ncol].rearrange("p (h x) -> p h x", x=64)
                        a3 = t1[:, :ncol].rearrange("p (h x) -> p h x", x=64)
                        b3 = t2[:, :ncol].rearrange("p (h x) -> p h x", x=64)
                        K.V(lambda: nc.vector.tensor_tensor(out=a3, in0=x3, in1=bc(cos_ap.unsqueeze(1), [128, H, 64]), op=ALU.mult),
                            r=[xs, ropet], w=[t1])
                        K.V(lambda: nc.vector.tensor_tensor(out=b3, in0=w3, in1=bc(sin_ap.unsqueeze(1), [128, H, 64]), op=ALU.mult),
                            r=[xsw, ropet], w=[t2])
                        K.V(lambda: nc.vector.tensor_tensor(out=out_t[:, ooff:ooff + ncol], in0=t1[:, :ncol], in1=t2[:, :ncol], op=ALU.add),
                            r=[t1, t2], w=[out_t])

                    deferred = []
                    pcount = [0]

                    def flush(all_=False):
                        while deferred and (all_ or deferred[0][0] <= pcount[0] - 2):
                            _, args = deferred.pop(0)
                            tr_store_now(*args)

                    def tr_store(*args):
                        deferred.append((pcount[0], args))

                    def tr_store_now(src_t, nblk, dst_t, dram_view):
                        s = psti[0] % 2
                        psti[0] += 1
                        transposes(lambda i: src_t[:, i * 128:(i + 1) * 128], nblk, pst[:, s], pstr[s], [src_t])
                        K.V(lambda: nc.vector.tensor_copy(out=dst_t[:, :nblk, :], in_=pst[:, s, :nblk, :]), r=[pstr[s]], w=[dst_t])
                        K.dma(K.sp, dram_view, dst_t[:, :nblk, :], r=[dst_t], dres=dst_t)

                    for n in range(NT):
                        t0 = n * 128
                        s = n % NB
                        rp = ropes[n % 2]
                        if n == 0:
                            K.dma(K.sp, rp[:], D["rope"][0], w=[rp], dres=rp)
                        if n + 1 < NT:
                            K.dma(K.sp, ropes[(n + 1) % 2][:], D["rope"][n + 1], w=[ropes[(n + 1) % 2]], dres=ropes[(n + 1) % 2])
                        cosR, sinR, cosA, sinA = rp[:, 0:64], rp[:, 64:128], rp[:, 128:192], rp[:, 192:256]
                        b = proj(n, 0)
                        rope(b, 512, 32, cosR, sinR, 1.0, o_q[s], 0, rp)
                        tr_store(o_q[s], 4, tq[s], rqT.rearrange("(c p) t -> p c t", p=128)[:, :, t0:t0 + 128])
                        b = proj(n, 1)
                        rope(b, 512, 32, cosR, sinR, 0.125, o_k[s], 0, rp)
                        K.dma(K.sp, rk[t0:t0 + 128, :], o_k[s][:], r=[o_k[s]], dres=o_k[s])
                        tr_store(o_k[s], 4, tk[s], rkT.rearrange("(c p) t -> p c t", p=128)[:, :, t0:t0 + 128])
                        for hh in range(2):
                            b = proj(n, 2 + hh)
                            K.A(lambda: nc.scalar.copy(out=o_v[s][:, hh * 512:(hh + 1) * 512], in_=pb[:, b, :]), r=[pres[b]], w=[o_v[s]])
                        K.dma(K.sp, rv[t0:t0 + 128, :], o_v[s][:], r=[o_v[s]], dres=o_v[s])
                        for hh in range(2):
                            b = proj(n, 4 + hh)
                            K.A(lambda: nc.scalar.activation(out=o_g[s][:, hh * 512:(hh + 1) * 512], in_=pb[:, b, :], func=AF.Silu),
                                r=[pres[b]], w=[o_g[s]])
                        K.dma(K.sp, rg[t0:t0 + 128, :], o_g[s][:], r=[o_g[s]], dres=o_g[s])
                        for hh in range(2):
                            b = proj(n, 6 + hh)
                            rope(b, 512, 16, cosA, sinA, 0.125, o_cq[s], hh * 512, rp)
                        tr_store(o_cq[s], 8, tcq[s], aqT.rearrange("(c p) t -> p c t", p=128)[:, :, t0:t0 + 128])
                        b = proj(n, 8)
                        rope(b, 256, 16, cosA, sinA, 1.0, o_ck[s], 0, rp)
                        K.A(lambda: nc.scalar.copy(out=o_cv[s][:], in_=pb[:, b, 256:512]), r=[pres[b]], w=[o_cv[s]])
                        K.dma(K.sp, av[t0:t0 + 128, :], o_cv[s][:], r=[o_cv[s]], dres=o_cv[s])
                        tr_store(o_ck[s], 2, tck[s], akT.rearrange("(c p) t -> p c t", p=128)[:, :, t0:t0 + 128])
                    flush(True)

            chk("P2c")
            with K.phase():
                kT = K.sb("kT", [64, 4, T], BF16)
                vaug = K.sb("vaug", [128, NT, 4, 65], BF16)
                esk = K.sb("esk", [128, 16], F32)
                K.dma(K.sp, kT[:], akT.rearrange("(g d) t -> d g t", d=64), w=[kT], dres=kT)
                for g in range(4):
                    K.dma(K.sp, vaug[:, :, g, 0:64], av.rearrange("(n p) (g d) -> p n g d", p=128, d=64)[:, :, g, :], w=[vaug], dres=vaug)
                K.G(lambda: nc.gpsimd.memset(vaug[:, :, :, 64:65], 1.0), w=[vaug])
                K.dma(K.sp, esk[:], D["sink_rep"][:, l], w=[esk], dres=esk)
                K.A(lambda: nc.scalar.activation(out=esk[:], in_=esk[:], func=AF.Exp), r=[esk], w=[esk])
                qbs = [K.sb(f"qb{i}", [64, 2048], BF16) for i in range(2)]
                pTs = [K.sb(f"pT{i}", [128, 5, 512], BF16) for i in range(3)]
                ycs = [K.sb(f"yc{i}", [128, 1024], BF16) for i in range(2)]
                ycts = [K.sb(f"yct{i}", [128, 8, 128], BF16) for i in range(2)]
                dens = [K.sb(f"den{i}", [128, 4], F32) for i in range(2)]
                recs = [K.sb(f"rec{i}", [128, 4], F32) for i in range(2)]
                pb = K.ps("pb", [128, 4, 512], F32)
                pres = [K.res() for _ in range(4)]
                po = K.ps("po", [128, 2, 4, 128], F32)
                por = [K.res(), K.res()]
                pst = K.ps("pst", [128, 8, 128], BF16)
                pstr = K.res()
                aq3 = aqT.rearrange("(h d) t -> d h t", d=64)
                sbi = [0]

                def keys_of(n):
                    if n < 2:
                        return [(0, None), (1, None)]
                    keys = []
                    if n - 1 >= 2:
                        keys.append((n - 1, maskp))
                    keys.append((n, None))
                    if n + 1 < NT:
                        keys.append((n + 1, maskn))
                    return keys + [(0, None), (1, None)]

                def load_q(n):
                    qb = qbs[n % 2]
                    K.dma(K.sp, qb[:].rearrange("d (h t) -> d h t", t=128), aq3[:, :, n * 128:(n + 1) * 128], w=[qb], dres=qb)

                def emit_scores(n, g, gi):
                    qb = qbs[n % 2]
                    pT = pTs[gi % 3]
                    for si, (kt, m) in enumerate(keys_of(n)):
                        bk = sbi[0] % 4
                        sbi[0] += 1
                        K.P(lambda: nc.tensor.matmul(pb[:, bk, :], lhsT=kT[:, g, kt * 128:(kt + 1) * 128], rhs=qb[:, g * 512:(g + 1) * 512],
                                                     start=True, stop=True), r=[kT, qb], w=[pres[bk]])
                        K.A(lambda: nc.scalar.activation(out=pT[:, si, :], in_=pb[:, bk, :], func=AF.Exp), r=[pres[bk]], w=[pT])
                        if m is not None:
                            pv = pT[:, si, :].rearrange("p (r i) -> p r i", i=128)
                            K.G(lambda: nc.gpsimd.tensor_tensor(out=pv, in0=pv, in1=bc(m[:, :].unsqueeze(1), [128, 4, 128]), op=ALU.mult),
                                r=[pT, m], w=[pT])

                def emit_pv(n, g, gi):
                    pT = pTs[gi % 3]
                    pos = gi % 2
                    yc = ycs[n % 2]
                    den, rec = dens[gi % 2], recs[gi % 2]
                    keys = keys_of(n)
                    for r_ in range(4):
                        for si, (kt, m) in enumerate(keys):
                            K.P(lambda: nc.tensor.matmul(po[:, pos, r_, 0:65], lhsT=pT[:, si, r_ * 128:(r_ + 1) * 128], rhs=vaug[:, kt, g, :],
                                                         start=(si == 0), stop=(si == len(keys) - 1)),
                                r=[pT, vaug], w=[por[pos]], inc=(si == len(keys) - 1))
                    K.V(lambda: nc.vector.tensor_tensor(out=den[:], in0=po[:, pos, :, 64], in1=esk[:, g * 4:(g + 1) * 4], op=ALU.add),
                        r=[por[pos], esk], w=[den])
                    K.V(lambda: nc.vector.reciprocal(out=rec[:], in_=den[:]), r=[den], w=[rec])
                    K.V(lambda: nc.vector.tensor_tensor(out=yc[:, g * 256:(g + 1) * 256].rearrange("p (r d) -> p r d", d=64),
                                                        in0=po[:, pos, :, 0:64], in1=bc(rec[:, :].unsqueeze(2), [128, 4, 64]), op=ALU.mult),
                        r=[por[pos], rec], w=[yc])
                    if g == 3:
                        t0 = n * 128
                        transposes(lambda i: yc[:, i * 128:(i + 1) * 128], 8, pst, pstr, [yc])
                        yct = ycts[n % 2]
                        K.V(lambda: nc.vector.tensor_copy(out=yct[:], in_=pst[:]), r=[pstr], w=[yct])
                        K.dma(K.sp, ycT.rearrange("(c p) t -> p c t", p=128)[:, :, t0:t0 + 128], yct[:], r=[yct], dres=yct)

                load_q(0)
                pending = None
                gi = 0
                for n in range(NT):
                    if n + 1 < NT:
                        load_q(n + 1)
                    for g in range(4):
                        emit_scores(n, g, gi)
                        if pending is not None:
                            emit_pv(*pending)
                        pending = (n, g, gi)
                        gi += 1
                emit_pv(*pending)

            chk("P3")
            with K.phase():
                lr = K.sb("lr", [128, 2, 8], F32)
                lS = K.sb("lS", [128, 2, 4], F32)
                K.dma(K.sp, lr[:], D["ret_lam_rep"][:, l], w=[lr], dres=lr)
                K.dma(K.sp, lS[:], D["ret_lam_S"][:, l], w=[lS], dres=lS)
                for tt in (lr, lS):
                    K.A(lambda: nc.scalar.activation(out=tt[:], in_=tt[:], func=AF.Exp, scale=-1.0), r=[tt], w=[tt])
                    K.A(lambda: nc.scalar.activation(out=tt[:], in_=tt[:], func=AF.Ln, bias=1.0), r=[tt], w=[tt])
                    K.V(lambda: nc.vector.tensor_scalar(out=tt[:], in0=tt[:], scalar1=-1.0, scalar2=None, op0=ALU.mult), r=[tt], w=[tt])
                G128 = K.sb("G128", [128, 2, 4], F32)
                K.A(lambda: nc.scalar.activation(out=G128[:], in_=lS[:], func=AF.Exp, scale=128.0), r=[lS], w=[G128])
                dec = K.sb("dec", [128, 4, 8], F32)
                cv = lambda j: cst[:, C_CV + j:C_CV + j + 1]
                K.A(lambda: nc.scalar.activation(out=dec[:, 0, :], in_=lr[:, 0, :], func=AF.Exp, scale=cv(1)), r=[lr, cst], w=[dec])
                K.A(lambda: nc.scalar.activation(out=dec[:, 1, :], in_=lr[:, 1, :], func=AF.Exp, scale=cv(0)), r=[lr, cst], w=[dec])
                K.A(lambda: nc.scalar.activation(out=dec[:, 2, :], in_=lr[:, 0, :], func=AF.Exp, scale=cv(2)), r=[lr, cst], w=[dec])
                K.A(lambda: nc.scalar.activation(out=dec[:, 3, :], in_=lr[:, 1, :], func=AF.Exp, scale=cv(3)), r=[lr, cst], w=[dec])
                DT = K.sb("DT", [128, 8, 128], F32)
                e1 = K.sb("e1", [128, 128], F32)
                e2 = K.sb("e2", [128, 128], F32)
                for h in range(8):
                    K.A(lambda: nc.scalar.activation(out=e1[:], in_=cst[:, C_R1:C_R1 + 128], func=AF.Exp, scale=lr[:, 0, h:h + 1]), r=[cst, lr], w=[e1])
                    K.A(lambda: nc.scalar.activation(out=e2[:], in_=cst[:, C_R2:C_R2 + 128], func=AF.Exp, scale=lr[:, 1, h:h + 1]), r=[cst, lr], w=[e2])
                    K.V(lambda: nc.vector.tensor_tensor(out=e1[:], in0=e1[:], in1=cst[:, C_M1:C_M1 + 128], op=ALU.mult), r=[e1, cst], w=[e1])
                    K.V(lambda: nc.vector.tensor_tensor(out=e2[:], in0=e2[:], in1=cst[:, C_M2:C_M2 + 128], op=ALU.mult), r=[e2, cst], w=[e2])
                    K.V(lambda: nc.vector.tensor_tensor(out=e1[:], in0=e1[:], in1=e2[:], op=ALU.add), r=[e1, e2], w=[e1])
                    K.V(lambda: nc.vector.tensor_tensor(out=DT[:, h, :], in0=e1[:], in1=cst[:, C_I2:C_I2 + 128], op=ALU.add), r=[e1, cst], w=[DT])
                Sst = K.sb("Sst", [128, 4, 128], F32)
                Sbf = K.sb("Sbf", [128, 4, 128], BF16)
                Stm = K.sb("Stm", [128, 4, 128], F32)
                kts = [K.sb(f"kt{i}", [128, 512], BF16) for i in range(2)]
                vts = [K.sb(f"vt{i}", [128, 1024], BF16) for i in range(2)]
                kd = K.sb("kd", [128, 512], BF16)
                pb = K.ps("pb", [128, 7, 512], F32)
                pres = [K.res() for _ in range(7)]
                pst4 = K.ps("pst4", [128, 8, 128], BF16)
                pst4r = K.res()

                def state_update(d, kt_, vt_, bank):
                    K.G(lambda: nc.gpsimd.tensor_tensor(out=kd[:].rearrange("p (h x) -> p h x", x=64),
                                                        in0=kt_[:].rearrange("p (h x) -> p h x", x=64),
                                                        in1=bc(dec[:, d, :].unsqueeze(2), [128, 8, 64]), op=ALU.mult),
                        r=[kt_, dec], w=[kd])
                    for h in range(8):
                        K.P(lambda: nc.tensor.matmul(pb[(h % 2) * 64:(h % 2) * 64 + 64, bank, (h // 2) * 128:(h // 2) * 128 + 128],
                                                     lhsT=kd[:, h * 64:(h + 1) * 64], rhs=vt_[:, h * 128:(h + 1) * 128], start=True, stop=True),
                            r=[kd, vt_], w=[pres[bank]], inc=(h == 7))
                    K.V(lambda: nc.vector.tensor_tensor(out=Stm[:], in0=Sst[:], in1=bc(G128[:, d, :].unsqueeze(2), [128, 4, 128]), op=ALU.mult),
                        r=[Sst, G128], w=[Stm])
                    K.V(lambda: nc.vector.tensor_tensor(out=Sst[:], in0=Stm[:], in1=pb[:, bank, :].rearrange("p (a e) -> p a e", e=128), op=ALU.add),
                        r=[Stm, pres[bank]], w=[Sst])
                    K.G(lambda: nc.gpsimd.tensor_copy(out=Sbf[:], in_=Sst[:]), r=[Sst], w=[Sbf])

                K.V(lambda: nc.vector.memset(Sst[:], 0.0), w=[Sst])
                K.V(lambda: nc.vector.memset(Sbf[:], 0.0), w=[Sbf])
                def loads1(n_):
                    K.dma(K.sp, kts[n_ % 2][:], rk[n_ * 128:(n_ + 1) * 128, :], w=[kts[n_ % 2]], dres=kts[n_ % 2])
                    K.dma(K.sp, vts[n_ % 2][:], rv[n_ * 128:(n_ + 1) * 128, :], w=[vts[n_ % 2]], dres=vts[n_ % 2])
                loads1(0)
                for n in range(NT):
                    t0 = n * 128
                    kt_, vt_ = kts[n % 2], vts[n % 2]
                    if n + 1 < NT:
                        loads1(n + 1)
                    K.dma(K.sp, SfD[n].rearrange("p (a e) -> p a e", e=128), Sbf[:], r=[Sbf], dres=Sbf)
                    state_update(0, kt_, vt_, n % 2)
                K.barrier()
                qTs = [K.sb(f"qT{i}", [128, 4, 128], BF16) for i in range(2)]
                kTs = [K.sb(f"kTt{i}", [128, 4, 128], BF16) for i in range(2)]
                gts = [K.sb(f"gt{i}", [128, 1024], BF16) for i in range(3)]
                Sfs = [K.sb(f"Sf{i}", [128, 4, 128], BF16) for i in range(2)]
                AT_l = [K.sb(f"AT{i}", [128, 8, 128], BF16) for i in range(2)]
                ysb_l = [K.sb(f"ysb{i}", [128, 1024], F32) for i in range(2)]
                ytm_l = [K.sb(f"ytm{i}", [128, 1024], F32) for i in range(2)]
                ysq_l = [K.sb(f"ysq{i}", [128, 1024], F32) for i in range(2)]
                s1_l = [K.sb(f"s1{i}", [128, 8], F32) for i in range(2)]
                s2_l = [K.sb(f"s2{i}", [128, 8], F32) for i in range(2)]
                mu_l = [K.sb(f"mu{i}", [128, 8], F32) for i in range(2)]
                var_l = [K.sb(f"var{i}", [128, 8], F32) for i in range(2)]
                ybs = [K.sb(f"yb{i}", [128, 1024], BF16) for i in range(2)]
                ybt = [K.sb(f"ybt{i}", [128, 8, 128], BF16) for i in range(2)]
                K.V(lambda: nc.vector.memset(Sst[:], 0.0), w=[Sst])
                K.V(lambda: nc.vector.memset(Sbf[:], 0.0), w=[Sbf])
                order = [1, 0] + list(range(NT - 1, 1, -1))

                def loads2(it_):
                    n_ = order[it_]
                    s_ = it_ % 2
                    t0_ = n_ * 128
                    K.dma(K.sp, qTs[s_][:], rqT.rearrange("(c p) t -> p c t", p=128)[:, :, t0_:t0_ + 128], w=[qTs[s_]], dres=qTs[s_])
                    K.dma(K.sp, kTs[s_][:], rkT.rearrange("(c p) t -> p c t", p=128)[:, :, t0_:t0_ + 128], w=[kTs[s_]], dres=kTs[s_])
                    K.dma(K.sp, kts[s_][:], rk[t0_:t0_ + 128, :], w=[kts[s_]], dres=kts[s_])
                    K.dma(K.sp, vts[s_][:], rv[t0_:t0_ + 128, :], w=[vts[s_]], dres=vts[s_])
                    K.dma(K.sp, Sfs[s_][:], SfD[n_].rearrange("p (a e) -> p a e", e=128), w=[Sfs[s_]], dres=Sfs[s_])
                    K.dma(K.sp, gts[it_ % 3][:], rg[t0_:t0_ + 128, :], w=[gts[it_ % 3]], dres=gts[it_ % 3])
                def stageA(it):
                    n = order[it]
                    s = it % 2
                    kt_, vt_, qT_, kT_, Sf_ = kts[s], vts[s], qTs[s], kTs[s], Sfs[s]
                    AT, ysb = AT_l[s], ysb_l[s]
                    for h in range(8):
                        po_ = (h % 2) * 64
                        sl = slice((h // 2) * 128, (h // 2) * 128 + 128)
                        K.P(lambda: nc.tensor.matmul(pb[:, h % 2, sl], lhsT=kT_[po_:po_ + 64, h // 2, :],
                                                     rhs=qT_[po_:po_ + 64, h // 2, :], start=True, stop=True),
                            r=[kT_, qT_], w=[pres[h % 2]])
                    for par in range(2):
                        K.V(lambda: nc.vector.tensor_tensor(out=AT[:, par::2, :],
                                                            in0=pb[:, par, :].rearrange("p (a e) -> p a e", e=128),
                                                            in1=DT[:, par::2, :], op=ALU.mult),
                            r=[pres[par], DT], w=[AT])
                    for h in range(8):
                        sl = slice((h // 2) * 128, (h // 2) * 128 + 128)
                        K.P(lambda: nc.tensor.matmul(pb[:, 2 + h % 2, sl], lhsT=AT[:, h, :], rhs=vt_[:, h * 128:(h + 1) * 128], start=True, stop=True),
                            r=[AT, vt_], w=[pres[2 + h % 2]])
                    for h in range(8):
                        po_ = (h % 2) * 64
                        sl = slice((h // 2) * 128, (h // 2) * 128 + 128)
                        K.P(lambda: nc.tensor.matmul(pb[:, 4 + h % 2, sl], lhsT=qT_[po_:po_ + 64, h // 2, :], rhs=Sf_[po_:po_ + 64, h // 2, :],
                                                     start=True, stop=True), r=[qT_, Sf_], w=[pres[4 + h % 2]])
                    for h in range(8):
                        po_ = (h % 2) * 64
                        sl = slice((h // 2) * 128, (h // 2) * 128 + 128)
                        K.P(lambda: nc.tensor.matmul(pb[:, h % 2, sl], lhsT=qT_[po_:po_ + 64, h // 2, :], rhs=Sbf[po_:po_ + 64, h // 2, :],
                                                     start=True, stop=True), r=[qT_, Sbf], w=[pres[h % 2]])
                    for par in range(2):
                        hs_ = slice(par * 512, (par + 1) * 512)
                        K.A(lambda: nc.scalar.copy(out=ysb[:, hs_], in_=pb[:, 2 + par, :]), r=[pres[2 + par]], w=[ysb])
                    for h in range(8):
                        par, pr = h % 2, h // 2
                        cs_ = slice(par * 512 + pr * 128, par * 512 + pr * 128 + 128)
                        ps_ = slice(pr * 128, pr * 128 + 128)
                        K.V(lambda: nc.vector.scalar_tensor_tensor(out=ysb[:, cs_], in0=pb[:, 4 + par, ps_], scalar=dec[:, 2, h:h + 1],
                                                                   in1=ysb[:, cs_], op0=ALU.mult, op1=ALU.add),
                            r=[pres[4 + par], dec, ysb], w=[ysb])
                        K.V(lambda: nc.vector.scalar_tensor_tensor(out=ysb[:, cs_], in0=pb[:, par, ps_], scalar=dec[:, 3, h:h + 1],
                                                                   in1=ysb[:, cs_], op0=ALU.mult, op1=ALU.add),
                            r=[pres[par], dec, ysb], w=[ysb])
                    state_update(1, kt_, vt_, 6)
                def stageB(it):
                    n = order[it]
                    t0 = n * 128
                    s = it % 2
                    gt_ = gts[it % 3]
                    ysb, ysq, s1, s2, mu, var = ysb_l[s], ysq_l[s], s1_l[s], s2_l[s], mu_l[s], var_l[s]
                    y3 = ysb[:].rearrange("p (h e) -> p h e", e=128)
                    for hp in range(8):
                        hsl = slice(hp * 128, (hp + 1) * 128)
                        K.A(lambda: nc.scalar.activation(out=ysq[:, hsl], in_=ysb[:, hsl], func=AF.Identity, accum_out=s1[:, hp:hp + 1]),
                            r=[ysb], w=[ysq, s1])
                        K.A(lambda: nc.scalar.activation(out=ysq[:, hsl], in_=ysb[:, hsl], func=AF.Square, accum_out=s2[:, hp:hp + 1]),
                            r=[ysb], w=[ysq, s2])
                    K.V(lambda: nc.vector.tensor_scalar(out=mu[:], in0=s1[:], scalar1=1.0 / 128.0, scalar2=None, op0=ALU.mult), r=[s1], w=[mu])
                    K.V(lambda: nc.vector.tensor_tensor(out=var[:], in0=mu[:], in1=mu[:], op=ALU.mult), r=[mu], w=[var])
                    K.V(lambda: nc.vector.scalar_tensor_tensor(out=var[:], in0=s2[:], scalar=1.0 / 128.0, in1=var[:], op0=ALU.mult, op1=ALU.subtract),
                        r=[s2, var], w=[var])
                    K.A(lambda: nc.scalar.activation(out=var[:], in_=var[:], func=AF.Sqrt, bias=epsT[:, 0:1]), r=[var, epsT], w=[var])
                    K.V(lambda: nc.vector.reciprocal(out=var[:], in_=var[:]), r=[var], w=[var])
                    K.G(lambda: nc.gpsimd.tensor_tensor(out=y3, in0=y3, in1=bc(mu[:, :].unsqueeze(2), [128, 8, 128]), op=ALU.subtract),
                        r=[ysb, mu], w=[ysb])
                    yb_ = ybs[s]
                    for h in range(8):
                        hp = (h % 2) * 4 + h // 2
                        K.V(lambda: nc.vector.scalar_tensor_tensor(out=yb_[:, h * 128:(h + 1) * 128], in0=ysb[:, hp * 128:(hp + 1) * 128],
                                                                   scalar=var[:, hp:hp + 1], in1=gt_[:, h * 128:(h + 1) * 128],
                                                                   op0=ALU.mult, op1=ALU.mult),
                            r=[ysb, var, gt_], w=[yb_])
                    for i in range(8):
                        K.P(lambda: nc.tensor.transpose(out=pst4[:, i, :], in_=yb_[:, i * 128:(i + 1) * 128], identity=identb[:]),
                            r=[yb_, identb], w=[pst4r], inc=(i == 7))
                    K.A(lambda: nc.scalar.copy(out=ybt[s][:], in_=pst4[:]), r=[pst4r], w=[ybt[s]])
                    K.dma(K.sp, ybT.rearrange("(c p) t -> p c t", p=128)[:, :, t0:t0 + 128], ybt[s][:], r=[ybt[s]], dres=ybt[s])

                loads2(0)
                for it in range(len(order)):
                    if it + 1 < len(order):
                        loads2(it + 1)
                    stageA(it)
                    if it >= 1:
                        stageB(it - 1)
                stageB(len(order) - 1)

            chk("P4")

            with K.phase():
                Ws = []
                for nm in ("w_ba", "w_bb", "w_bc", "w_out"):
                    wt = K.sb(nm, [128, 8, 1024], BF16)
                    for hh in range(2):
                        K.dma(K.pool, wt[:, :, hh * 512:(hh + 1) * 512],
                              D[nm][l].rearrange("(k p) n -> p k n", p=128)[:, :, hh * 512:(hh + 1) * 512], w=[wt], dres=wt)
                    Ws.append(wt)
                Wa, Wb, Wc, Wo = Ws
                yas = [K.sb(f"ya{i}", [128, 8, 512], BF16) for i in range(2)]
                ybs_ = [K.sb(f"ybm{i}", [128, 8, 512], BF16) for i in range(2)]
                ycs_ = [K.sb(f"ycm{i}", [128, 8, 512], BF16) for i in range(2)]
                sgs = [K.sb(f"sg{i}", [128, 24, 512], BF16) for i in range(1)] * 2
                hbs = [K.sb(f"hb{i}", [128, 8, 512], F32) for i in range(2)]
                mT = K.sb("mT", [128, 8, 512], BF16)
                m1 = K.sb("m1", [128, 512], F32)
                m2 = K.sb("m2", [128, 512], F32)
                m3 = K.sb("m3", [128, 512], F32)
                pb = K.ps("pb", [128, 8, 512], F32)
                pres = [K.res() for _ in range(8)]
                def loads5(bi_):
                    t0_, bs_ = TB[bi_]
                    s_ = bi_ % 2
                    K.dma(K.sp, yas[s_][:, :, :bs_], fm(yaT, t0_, bs_), w=[yas[s_]], dres=yas[s_])
                    K.dma(K.sp, ybs_[s_][:, :, :bs_], fm(ybT, t0_, bs_), w=[ybs_[s_]], dres=ybs_[s_])
                    K.dma(K.sp, ycs_[s_][:, :, :bs_], fm(ycT, t0_, bs_), w=[ycs_[s_]], dres=ycs_[s_])
                    K.dma(K.sp, hbs[s_][:, :, :bs_], fm(hT, t0_, bs_), w=[hbs[s_]], dres=hbs[s_])

                def loadsg(bi_):
                    t0_, bs_ = TB[bi_]
                    K.dma(K.sp, sgs[0][:, :, :bs_], sgT.rearrange("(k p) t -> p k t", p=128)[:, :, t0_:t0_ + bs_], w=[sgs[0]], dres=sgs[0])
                loads5(0)
                for bi, (t0, bs) in enumerate(TB):
                    s = bi % 2
                    v = 1 if bi == 0 else 0
                    loadsg(bi)
                    if bi + 1 < len(TB):
                        loads5(bi + 1)
                    for nn in range(8):
                        bks = []
                        for (W_, y_) in ((Wa, yas[s]), (Wb, ybs_[s]), (Wc, ycs_[s])):
                            b = K.bank()
                            bks.append(b)
                            for k in range(8):
                                K.P(lambda: nc.tensor.matmul(pb[:, b, :bs], lhsT=W_[:, k, nn * 128:(nn + 1) * 128], rhs=y_[:, k, :bs],
                                                             start=(k == 0), stop=(k == 7)), r=[W_, y_], w=[pres[b]], inc=(k == 7))
                        K.V(lambda: nc.vector.tensor_tensor(out=m1[:, :bs], in0=pb[:, bks[0], :bs], in1=sgs[s][:, nn, :bs], op=ALU.mult),
                            r=[pres[bks[0]], sgs[s]], w=[m1])
                        K.V(lambda: nc.vector.tensor_tensor(out=m2[:, :bs], in0=pb[:, bks[1], :bs], in1=sgs[s][:, 8 + nn, :bs], op=ALU.mult),
                            r=[pres[bks[1]], sgs[s]], w=[m2])
                        K.V(lambda: nc.vector.tensor_tensor(out=m3[:, :bs], in0=pb[:, bks[2], :bs], in1=sgs[s][:, 16 + nn, :bs], op=ALU.mult),
                            r=[pres[bks[2]], sgs[s]], w=[m3])
                        K.G(lambda: nc.gpsimd.tensor_tensor(out=m1[:, :bs], in0=m1[:, :bs], in1=m2[:, :bs], op=ALU.add), r=[m1, m2], w=[m1])
                        K.G(lambda: nc.gpsimd.tensor_tensor(out=mT[:, nn, :bs], in0=m1[:, :bs], in1=m3[:, :bs], op=ALU.add), r=[m1, m3], w=[mT])
                    for nn in range(8):
                        b = K.bank()
                        for k in range(8):
                            K.P(lambda: nc.tensor.matmul(pb[:, b, :bs], lhsT=Wo[:, k, nn * 128:(nn + 1) * 128], rhs=mT[:, k, :bs],
                                                         start=(k == 0), stop=(k == 7)), r=[Wo, mT], w=[pres[b]], inc=(k == 7))
                        K.V(lambda: nc.vector.scalar_tensor_tensor(out=hbs[s][:, nn, :bs], in0=pb[:, b, :bs], scalar=mods(l, 2, nn, v),
                                                                   in1=hbs[s][:, nn, :bs], op0=ALU.mult, op1=ALU.add),
                            r=[pres[b], modT, hbs[s]], w=[hbs[s]])
                    K.dma(K.sp, fm(hT, t0, bs), hbs[s][:, :, :bs], r=[hbs[s]], dres=hbs[s])

            chk("P5")

            with K.phase():
                hbs = [K.sb(f"hb{i}", [128, 8, 512], F32) for i in range(2)]
                sqb = K.sb("sqb", [128, 8, 512], BF16)
                tmp = K.sb("tmp", [128, 8, 512], F32)
                v32 = K.sb("v32", [128, 8, 512], F32)
                rt = K.sb("rt", [128, 512], F32)
                rstd = K.sb("rstd", [128, 512], F32)
                wr = K.sb("wr", [128, 8, 36], F32)
                br = K.sb("br", [128, 36], F32)
                K.dma(K.sp, wr[:], D["w_rt"][:, l], w=[wr], dres=wr)
                K.dma(K.sp, br[:], D["b_rt"][:, l], w=[br], dres=br)
                run = K.sb("run", [128, 32], F32)
                K.V(lambda: nc.vector.memset(run[:], 0.0), w=[run])
                lg = K.sb("lg", [128, 36], F32)
                gmx = K.sb("gmx", [128, 1], F32)
                ngm = K.sb("ngm", [128, 1], F32)
                gex = K.sb("gex", [128, 4], F32)
                gsum = K.sb("gsum", [128, 1], F32)
                gw = K.sb("gw", [128, 1], F32)
                gmk = K.sb("gmk", [128, 4], F32)
                lem = K.sb("lem", [128, 32], F32)
                m8 = K.sb("m8", [128, 8], F32)
                mk1 = K.sb("mk1", [128, 32], F32)
                mk2 = K.sb("mk2", [128, 32], F32)
                mk12 = K.sb("mk12", [128, 32], BF16)
                dd = K.sb("dd", [128, 1], F32)
                posv = K.sb("posv", [128, 32], F32)
                sf = K.sb("sf", [128, 2], F32)
                junk = K.sb("junk", [128, 32], F32)
                rows = [K.sb(f"rows{i}", [128, 1024], BF16) for i in range(2)]
                pb = K.ps("pb", [128, 2, 512], F32)
                pres = [K.res(), K.res()]
                pr = K.ps("pr", [128, 2, 64], F32)
                prr = [K.res(), K.res()]
                pc = K.ps("pc", [128, 2, 32], F32)
                pcr = K.res()
                ptr = K.ps("ptr", [128, 2, 1024], F32)
                ptrr = [K.res(), K.res()]
                ti = 0
                for bi, (t0, bs) in enumerate(TB):
                    hb = hbs[bi % 2]
                    v = 1 if bi == 0 else 0
                    K.dma(K.sp, hb[:, :, :bs], fm(hT, t0, bs), w=[hb], dres=hb)
                    norm_block(hb, bs, sqb, tmp, rt, rstd, pb[:, bi % 2, :], pres[bi % 2])
                    for k in range(8):
                        K.A(lambda: nc.scalar.activation(out=v32[:, k, :bs], in_=tmp[:, k, :bs], func=AF.Identity,
                                                         bias=mods(l, 3, k, v), scale=gs2[:, k, v:v + 1]), r=[tmp, gs2, modT], w=[v32])
                    for tt in range(bs // 128):
                        n = t0 // 128 + tt
                        q = ti % 2
                        ti += 1
                        ts_ = slice(tt * 128, (tt + 1) * 128)
                        for k in range(8):
                            K.P(lambda: nc.tensor.matmul(pr[:, q, 0:36], lhsT=v32[:, k, ts_], rhs=wr[:, k, :], start=(k == 0), stop=(k == 7)),
                                r=[v32, wr], w=[prr[q]], inc=(k == 7))
                        for k in range(8):
                            K.P(lambda: nc.tensor.transpose(out=ptr[:, q, k * 128:(k + 1) * 128], in_=v32[:, k, ts_], identity=cst[:, C_ID:C_ID + 128]),
                                r=[v32, cst], w=[ptrr[q]], inc=(k == 7))
                        rw = rows[q]
                        K.A(lambda: nc.scalar.copy(out=rw[:], in_=ptr[:, q, :]), r=[ptrr[q]], w=[rw])
                        K.V(lambda: nc.vector.tensor_tensor(out=lg[:], in0=pr[:, q, 0:36], in1=br[:], op=ALU.add), r=[prr[q], br], w=[lg])
                        K.V(lambda: nc.vector.reduce_max(out=gmx[:], in_=lg[:, 0:4], axis=AX.X), r=[lg], w=[gmx])
                        K.V(lambda: nc.vector.tensor_scalar(out=ngm[:], in0=gmx[:], scalar1=-1.0, scalar2=None, op0=ALU.mult), r=[gmx], w=[ngm])
                        K.A(lambda: nc.scalar.activation(out=gex[:], in_=lg[:, 0:4], func=AF.Exp, bias=ngm[:, 0:1]), r=[lg, ngm], w=[gex])
                        K.V(lambda: nc.vector.reduce_sum(out=gsum[:], in_=gex[:], axis=AX.X), r=[gex], w=[gsum])
                        K.V(lambda: nc.vector.reciprocal(out=gw[:], in_=gsum[:]), r=[gsum], w=[gw])
                        K.V(lambda: nc.vector.tensor_scalar(out=gmk[:], in0=lg[:, 0:4], scalar1=gmx[:, 0:1], scalar2=None, op0=ALU.is_equal),
                            r=[lg, gmx], w=[gmk])
                        K.V(lambda: nc.vector.tensor_scalar(out=gex[:], in0=gmk[:], scalar1=1e30, scalar2=-1e30, op0=ALU.mult, op1=ALU.add),
                            r=[gmk], w=[gex])
                        K.V(lambda: nc.vector.tensor_tensor(out=lem[:].rearrange("p (g e) -> p g e", e=8),
                                                            in0=lg[:, 4:36].rearrange("p (g e) -> p g e", e=8),
                                                            in1=bc(gex[:, :].unsqueeze(2), [128, 4, 8]), op=ALU.add), r=[lg, gex], w=[lem])
                        K.V(lambda: nc.vector.max(out=m8[:], in_=lem[:]), r=[lem], w=[m8])
                        K.V(lambda: nc.vector.tensor_scalar(out=mk1[:], in0=lem[:], scalar1=m8[:, 0:1], scalar2=None, op0=ALU.is_equal),
                            r=[lem, m8], w=[mk1])
                        K.V(lambda: nc.vector.tensor_scalar(out=mk2[:], in0=lem[:], scalar1=m8[:, 1:2], scalar2=None, op0=ALU.is_equal),
                            r=[lem, m8], w=[mk2])
                        K.V(lambda: nc.vector.tensor_tensor(out=mk12[:], in0=mk1[:], in1=mk2[:], op=ALU.add), r=[mk1, mk2], w=[mk12])
                        K.V(lambda: nc.vector.tensor_tensor(out=dd[:], in0=m8[:, 0:1], in1=m8[:, 1:2], op=ALU.subtract), r=[m8], w=[dd])
                        K.A(lambda: nc.scalar.activation(out=dd[:], in_=dd[:], func=AF.Sigmoid), r=[dd], w=[dd])
                        K.V(lambda: nc.vector.tensor_tensor(out=wts[:, n, 0:1], in0=dd[:], in1=gw[:], op=ALU.mult), r=[dd, gw], w=[wts])
                        K.V(lambda: nc.vector.tensor_tensor(out=wts[:, n, 1:2], in0=gw[:], in1=wts[:, n, 0:1], op=ALU.subtract), r=[gw, wts], w=[wts])
                        K.P(lambda: nc.tensor.matmul(pc[:, 0, :], lhsT=trib[:], rhs=mk12[:], start=True, stop=True), r=[trib, mk12], w=[pcr], inc=False)
                        K.P(lambda: nc.tensor.matmul(pc[:, 1, :], lhsT=onesb[:], rhs=mk12[:], start=True, stop=True), r=[onesb, mk12], w=[pcr])
                        K.V(lambda: nc.vector.tensor_tensor(out=posv[:], in0=pc[:, 0, :], in1=run[:], op=ALU.add), r=[pcr, run], w=[posv])
                        K.V(lambda: nc.vector.tensor_tensor(out=posv[:], in0=posv[:], in1=cst[:, C_EB:C_EB + 32], op=ALU.add), r=[posv, cst], w=[posv])
                        K.V(lambda: nc.vector.tensor_tensor(out=run[:], in0=run[:], in1=pc[:, 1, :], op=ALU.add), r=[run, pcr], w=[run])
                        K.V(lambda: nc.vector.tensor_tensor(out=junk[:], in0=mk1[:], in1=posv[:], op=ALU.mult), r=[mk1, posv], w=[junk])
                        K.V(lambda: nc.vector.reduce_sum(out=sf[:, 0:1], in_=junk[:], axis=AX.X), r=[junk], w=[sf])
                        K.V(lambda: nc.vector.tensor_tensor(out=junk[:], in0=mk2[:], in1=posv[:], op=ALU.mult), r=[mk2, posv], w=[junk])
                        K.V(lambda: nc.vector.reduce_sum(out=sf[:, 1:2], in_=junk[:], axis=AX.X), r=[junk], w=[sf])
                        K.V(lambda: nc.vector.tensor_scalar(out=sf[:], in0=sf[:], scalar1=float(NSLOT - 1), scalar2=None, op0=ALU.min), r=[sf], w=[sf])
                        K.V(lambda: nc.vector.tensor_copy(out=slots[:, n, :], in_=sf[:]), r=[sf], w=[slots])
                        for j in range(2):
                            K.idma(xslot[:, :], rw[:, :], bass.IndirectOffsetOnAxis(ap=slots[:, n, j:j + 1], axis=0), None,
                                   r=[rw, slots], dres=rw)

            chk("P6")
            with K.phase():
                SG = 512
                NSG = CAP // SG
                NST = SG // 128
                wgs = [K.sb(f"ewg{i}", [128, 8, 512], BF16) for i in range(2)]
                wus = [K.sb(f"ewu{i}", [128, 8, 512], BF16) for i in range(2)]
                wds = [K.sb(f"ewd{i}", [128, 4, 1024], BF16) for i in range(2)]
                xrs = [K.sb(f"xr{i}", [128, NST, 1024], BF16) for i in range(2)]
                xTs = [K.sb(f"xT{i}", [128, 8, SG], BF16) for i in range(2)]
                hTs_ = [K.sb(f"hTe{i}", [128, 4, SG], BF16) for i in range(2)]
                sacts = [K.sb(f"sact{i}", [128, SG], F32) for i in range(2)]
                sai = 0
                yss = [K.sb(f"ys{i}", [128, NST, 1024], BF16) for i in range(2)]
                pb = K.ps("pb", [128, 6, 512], F32)
                pres = [K.res() for _ in range(6)]
                pst = K.ps("pst", [128, 2, 8, 128], BF16)
                pstr = [K.res(), K.res()]
                bki = [0]

                def nb():
                    bki[0] = (bki[0] + 1) % 6
                    return bki[0]
                yi = 0
                pi = 0
                xi = 0
                for e in range(32):
                    s = e % 2
                    wg, wu, wd = wgs[s], wus[s], wds[s]
                    K.dma(K.pool, wg[:], D["e_wg"][l, e].rearrange("(k p) n -> p k n", p=128), w=[wg], dres=wg)
                    K.dma(K.pool, wu[:], D["e_wu"][l, e].rearrange("(k p) n -> p k n", p=128), w=[wu], dres=wu)
                    K.dma(K.pool, wd[:], D["e_wd"][l, e].rearrange("(k p) n -> p k n", p=128), w=[wd], dres=wd)
                    for sgi in range(NSG):
                        base = e * CAP + sgi * SG
                        xr = xrs[xi % 2]
                        xT = xTs[xi % 2]
                        hT_ = hTs_[xi % 2]
                        if xi == 0:
                            K.dma(K.sp, xr[:], xslot[base:base + SG, :].rearrange("(a p) f -> p a f", p=128), w=[xr], dres=xr)
                        xi += 1
                        if xi < 32 * NSG:
                            nbase = (xi // NSG) * CAP + (xi % NSG) * SG
                            xrn = xrs[xi % 2]
                            K.dma(K.sp, xrn[:], xslot[nbase:nbase + SG, :].rearrange("(a p) f -> p a f", p=128), w=[xrn], dres=xrn)
                        for a in range(NST):
                            q = pi % 2
                            pi += 1
                            transposes(lambda i: xr[:, a, i * 128:(i + 1) * 128], 8, pst[:, q], pstr[q], [xr])
                            K.V(lambda: nc.vector.tensor_copy(out=xT[:, :, a * 128:(a + 1) * 128], in_=pst[:, q]), r=[pstr[q]], w=[xT])
                        for jc in range(4):
                            b1 = nb()
                            for k in range(8):
                                K.P(lambda: nc.tensor.matmul(pb[:, b1, :SG], lhsT=wg[:, k, jc * 128:(jc + 1) * 128], rhs=xT[:, k, :],
                                                             start=(k == 0), stop=(k == 7)), r=[wg, xT], w=[pres[b1]], inc=(k == 7))
                            b2 = nb()
                            for k in range(8):
                                K.P(lambda: nc.tensor.matmul(pb[:, b2, :SG], lhsT=wu[:, k, jc * 128:(jc + 1) * 128], rhs=xT[:, k, :],
                                                             start=(k == 0), stop=(k == 7)), r=[wu, xT], w=[pres[b2]], inc=(k == 7))
                            sact = sacts[sai % 2]
                            sai += 1
                            K.A(lambda: nc.scalar.activation(out=sact[:], in_=pb[:, b1, :SG], func=AF.Silu), r=[pres[b1]], w=[sact])
                            K.V(lambda: nc.vector.tensor_tensor(out=hT_[:, jc, :], in0=sact[:], in1=pb[:, b2, :SG], op=ALU.mult),
                                r=[sact, pres[b2]], w=[hT_])
                        ys = yss[yi % 2]
                        yi += 1
                        for a in range(NST):
                            for nh in range(2):
                                b = nb()
                                for jc in range(4):
                                    K.P(lambda: nc.tensor.matmul(pb[:, b, :], lhsT=hT_[:, jc, a * 128:(a + 1) * 128], rhs=wd[:, jc, nh * 512:(nh + 1) * 512],
                                                                 start=(jc == 0), stop=(jc == 3)), r=[hT_, wd], w=[pres[b]], inc=(jc == 3))
                                K.A(lambda: nc.scalar.copy(out=ys[:, a, nh * 512:(nh + 1) * 512], in_=pb[:, b, :]), r=[pres[b]], w=[ys])
                        K.dma(K.sp, yslot[base:base + SG, :].rearrange("(a p) f -> p a f", p=128), ys[:], r=[ys], dres=ys)

            chk("P7")
            with K.phase():
                y1s = [K.sb(f"y1_{i}", [128, 1024], BF16) for i in range(2)]
                y2s = [K.sb(f"y2_{i}", [128, 1024], BF16) for i in range(2)]
                yfs = [K.sb(f"yf_{i}", [128, 1024], F32) for i in range(2)]
                ygs = [K.sb(f"yg_{i}", [128, 1024], F32) for i in range(2)]
                hts = [K.sb(f"ht{i}", [128, 8, 128], F32) for i in range(2)]
                ptr = K.ps("ptr", [128, 2, 8, 128], F32)
                ptrr = [K.res(), K.res()]
                def loads8(n_):
                    s_ = n_ % 2
                    K.idma(y1s[s_][:, :], yslot[:, :], None, bass.IndirectOffsetOnAxis(ap=slots[:, n_, 0:1], axis=0), r=[slots], w=[y1s[s_]], dres=y1s[s_])
                    K.idma(y2s[s_][:, :], yslot[:, :], None, bass.IndirectOffsetOnAxis(ap=slots[:, n_, 1:2], axis=0), r=[slots], w=[y2s[s_]], dres=y2s[s_])
                    K.dma(K.sp, hts[s_][:], fm(hT, n_ * 128, 128), w=[hts[s_]], dres=hts[s_])
                loads8(0)
                for n in range(NT):
                    t0 = n * 128
                    s = n % 2
                    v = 1 if n < 2 else 0
                    y1, y2, ht = y1s[s], y2s[s], hts[s]
                    if n + 1 < NT:
                        loads8(n + 1)
                    yf, yg = yfs[s], ygs[s]
                    K.V(lambda: nc.vector.tensor_scalar(out=yf[:], in0=y1[:], scalar1=wts[:, n, 0:1], scalar2=None, op0=ALU.mult), r=[y1, wts], w=[yf])
                    K.V(lambda: nc.vector.scalar_tensor_tensor(out=yf[:], in0=y2[:], scalar=wts[:, n, 1:2], in1=yf[:], op0=ALU.mult, op1=ALU.add),
                        r=[y2, wts, yf], w=[yf])
                    for k in range(8):
                        K.P(lambda: nc.tensor.transpose(out=ptr[:, s, k, :], in_=yf[:, k * 128:(k + 1) * 128], identity=cst[:, C_ID:C_ID + 128]),
                            r=[yf, cst], w=[ptrr[s]], inc=(k == 7))
                    K.V(lambda: nc.vector.tensor_tensor(out=yg[:].rearrange("p (k t) -> p k t", t=128), in0=ptr[:, s],
                                                        in1=bc(modv(l, 5, v).unsqueeze(2), [128, 8, 128]), op=ALU.mult),
                        r=[ptrr[s], modT], w=[yg])
                    K.G(lambda: nc.gpsimd.tensor_tensor(out=ht[:], in0=ht[:], in1=yg[:].rearrange("p (k t) -> p k t", t=128), op=ALU.add),
                        r=[ht, yg], w=[ht])
                    K.dma(K.sp, fm(hT, t0, 128), ht[:], r=[ht], dres=ht)

            chk("P8")
          except _Stop:
            break

        with K.phase():
            hbs = [K.sb(f"hb{i}", [128, 8, 512], F32) for i in range(2)]
            sqb = K.sb("sqb", [128, 8, 512], BF16)
            tmps = [K.sb(f"tmp{i}", [128, 8, 512], F32) for i in range(2)]
            rt = K.sb("rt", [128, 512], F32)
            rstd = K.sb("rstd", [128, 512], F32)
            pb = K.ps("pb", [128, 2, 512], F32)
            pres = [K.res(), K.res()]
            for bi in range(1, 9):
                t0, bs = TB[bi]
                hb, tmp = hbs[bi % 2], tmps[bi % 2]
                K.dma(K.sp, hb[:], fm(hT if n_layers > 0 else D["h0T"], t0, bs), w=[hb], dres=hb)
                norm_block(hb, bs, sqb, tmp, rt, rstd, pb[:, bi % 2, :], pres[bi % 2])
                K.V(lambda: nc.vector.tensor_tensor(out=tmp[:], in0=tmp[:], in1=bc(gfT[:, :].unsqueeze(2), [128, 8, 512]), op=ALU.mult),
                    r=[tmp, gfT], w=[tmp])
                K.dma(K.sp, OUT.rearrange("(k p) t -> p k t", p=128)[:, :, t0 - 256:t0 - 256 + bs], tmp[:], r=[tmp], dres=tmp)
    return nc


def _consts():
    c = np.zeros((128, 1412), np.float32)
    j = np.arange(128)[:, None].astype(np.float32)
    i = np.arange(128)[None, :].astype(np.float32)
    c[:, 0:128] = np.eye(128, dtype=np.float32)
    c[:, 128:256] = np.maximum(i - j, 0)
    c[:, 256:384] = (j < i)
    c[:, 384:512] = np.maximum(j - i, 0)
    c[:, 512:640] = (j > i)
    c[:, 640:768] = 2.0 * (i == j)
    c[:, 768:896] = (j >= i)
    c[:, 896:1024] = (j <= i)
    p = np.arange(128, dtype=np.float32)
    c[:, 1024] = p
    c[:, 1025] = 127 - p
    c[:, 1026] = p + 1
    c[:, 1027] = 128 - p
    c[:, 1028:1156] = (j < i)
    c[:, 1156:1188] = (np.arange(32, dtype=np.float32) * CAP)[None, :]
    c[:, 1188:1316] = 1.0
    return c


def _rope_tables():
    out = np.zeros((NT, 128, 256), np.float32)
    out[:, :, 0:64] = 1.0
    out[:, :, 128:192] = 1.0
    pos = np.arange(SEQ, dtype=np.float32)
    inv_r = (10000.0 ** (-(np.arange(0, 64, 2, dtype=np.float32) / 64.0))).astype(np.float32)
    ang = (pos[:, None] * inv_r[None, :]).astype(np.float32)
    cr, sr = np.cos(ang), np.sin(ang)
    inv_a = (10000.0 ** (-(np.arange(0, 32, 2, dtype=np.float32) / 32.0))).astype(np.float32)
    row = np.floor(pos / 64.0).astype(np.float32)
    col = (pos - row * 64.0).astype(np.float32)
    ar = (row[:, None] * inv_a[None, :]).astype(np.float32)
    ac = (col[:, None] * inv_a[None, :]).astype(np.float32)
    lat = np.zeros((SEQ, 256), np.float32)
    lat[:, 0:32] = cr
    lat[:, 32:64] = cr
    lat[:, 64:96] = -sr
    lat[:, 96:128] = sr
    lat[:, 128:144] = np.cos(ar)
    lat[:, 144:160] = np.cos(ar)
    lat[:, 160:176] = np.cos(ac)
    lat[:, 176:192] = np.cos(ac)
    lat[:, 192:208] = -np.sin(ar)
    lat[:, 208:224] = np.sin(ar)
    lat[:, 224:240] = -np.sin(ac)
    lat[:, 240:256] = np.sin(ac)
    out[2:] = lat.reshape(32, 128, 256)
    return out


def _fmv(a):
    a = np.asarray(a, np.float32)
    lead = a.shape[:-1]
    r = a.reshape(lead + (8, 128))
    r = np.moveaxis(r, -1, 0)
    return np.ascontiguousarray(r)


def prepare_inputs(inp):
    f = lambda a: np.ascontiguousarray(np.asarray(a, np.float32))
    shared = {}
    shared["w_mod"] = f(inp["w_mod"])
    shared["b_modT"] = np.ascontiguousarray(np.asarray(inp["b_mod"], np.float32).reshape(DEPTH, 48, 128).transpose(2, 0, 1))
    shared["g1T"] = _fmv(inp["norm1_g"])
    shared["g2T"] = _fmv(inp["norm2_g"])
    shared["gfT"] = _fmv(inp["final_norm_g"])
    shared["w_in"] = f(inp["w_in"])
    cw = np.asarray(inp["lru_conv_w"], np.float32)
    shared["conv_wT"] = np.ascontiguousarray(cw.reshape(DEPTH, 4, 8, 128).transpose(3, 0, 2, 1))
    shared["conv_bT"] = _fmv(inp["lru_conv_b"])
    for nm, key in (("wa_bd", "lru_wa"), ("wx_bd", "lru_wx")):
        w = np.asarray(inp[key], np.float32)
        bd = np.zeros((DEPTH, 8, 128, 2, 128), np.float32)
        for c in range(8):
            for hh in range(2):
                bd[:, c, hh * 64:(hh + 1) * 64, :, hh * 64:(hh + 1) * 64] = w[:, :, 2 * c + hh].transpose(0, 2, 1, 3)
        shared[nm] = bd
    shared["lru_baT"] = _fmv(inp["lru_ba"])
    shared["lru_bxT"] = _fmv(inp["lru_bx"])
    shared["lru_lamT"] = _fmv(inp["lru_lambda"])
    rl = np.asarray(inp["ret_lambda"], np.float32)
    shared["ret_lam_rep"] = np.ascontiguousarray(np.broadcast_to(rl[None], (128, DEPTH, 2, 8)))
    rs = np.zeros((128, DEPTH, 2, 4), np.float32)
    for pr in range(4):
        rs[0:64, :, :, pr] = rl[None, :, :, 2 * pr]
        rs[64:128, :, :, pr] = rl[None, :, :, 2 * pr + 1]
    shared["ret_lam_S"] = rs
    shared["sink_rep"] = np.ascontiguousarray(np.broadcast_to(np.asarray(inp["attn_sink"], np.float32)[None], (128, DEPTH, 16)))
    shared["w_ba"] = f(inp["w_branch_a"])
    shared["w_bb"] = f(inp["w_branch_b"])
    shared["w_bc"] = f(inp["w_branch_c"])
    shared["w_out"] = f(inp["w_out"])
    wr = np.concatenate([np.asarray(inp["router_group_w"], np.float32), np.asarray(inp["router_expert_w"], np.float32)], axis=-1)
    shared["w_rt"] = np.ascontiguousarray(wr.reshape(DEPTH, 8, 128, 36).transpose(2, 0, 1, 3))
    br = np.concatenate([np.asarray(inp["router_group_b"], np.float32), np.asarray(inp["router_expert_b"], np.float32)], axis=-1)
    shared["b_rt"] = np.ascontiguousarray(np.broadcast_to(br[None], (128, DEPTH, 36)))
    shared["e_wg"] = f(inp["expert_w_gate"])
    shared["e_wu"] = f(inp["expert_w_up"])
    shared["e_wd"] = f(inp["expert_w_down"])
    shared["consts"] = _consts()
    shared["rope"] = _rope_tables()
    x = np.asarray(inp["x"], np.float32)
    ctx = np.asarray(inp["ctx"], np.float32)
    c = np.asarray(inp["c"], np.float32)
    cc = np.asarray(inp["c_ctx"], np.float32)
    in_maps = []
    for b in range(N_CORES):
        m = dict(shared)
        m["h0T"] = np.ascontiguousarray(np.concatenate([ctx[b], x[b]], axis=0).T)
        cT = np.stack([c[b].reshape(8, 128).T, cc.reshape(8, 128).T], axis=-1)
        m["cT"] = np.ascontiguousarray(cT.astype(np.float32))
        in_maps.append(m)
    return in_maps


_NC_CACHE = {}


def kernel(**inputs):
    in_maps = prepare_inputs(inputs)
    if "nc" not in _NC_CACHE:
        _NC_CACHE["nc"] = build_program()
    res = run_bass_kernel_spmd(_NC_CACHE["nc"], in_maps, core_ids=list(range(N_CORES)))
    out = np.stack([np.ascontiguousarray(r["outT"].T) for r in res.results], axis=0)
    return out.astype(np.float32)
```
